# Optimizing a Trainium2 kernel written in Bass

```python
import math
import jax, jax.numpy as jnp
from jax import lax
import numpy as np

D_MODEL = 1024
BATCH = 8
SEQ = 2048
DEPTH = 4

ATTN_WIDTH = D_MODEL // 2
HEAD_DIM = 64
ATTN_HEADS = ATTN_WIDTH // HEAD_DIM
DILATED_PAIRS = ((128, 1), (512, 4), (2048, 16))
SSM_WIDTH = D_MODEL - ATTN_WIDTH
SSM_GROUP_CH = 16
SSM_GROUPS = SSM_WIDTH // SSM_GROUP_CH
SSM_STATE = 64
IN_WIDTH = 3 * ATTN_WIDTH + SSM_WIDTH
MOE_GROUPS = 4
EXPERTS_PER_GROUP = 4
N_EXPERTS = MOE_GROUPS * EXPERTS_PER_GROUP
EXPERT_FF = D_MODEL // 4
TOP_K_INNER = 2
RMS_EPS = 1e-6
DT_MIN = 1e-3
DT_MAX = 1e-1

kernel_name = "hymba_s5_dilated_hmoe_trunk"


def rmsnorm(x, g):
    xf = x.astype(jnp.float32)
    y = xf * lax.rsqrt(jnp.mean(xf * xf, axis=-1, keepdims=True) + RMS_EPS)
    return (y * g.astype(jnp.float32)).astype(x.dtype)


def dilated_branch(q, k, v, dil, span):
    B, S, H, Dh = q.shape
    M = S // dil
    nb = -(-M // span)
    Mp = nb * span

    def blocks(a):
        a = a.reshape(B, M, dil, H, Dh)
        a = jnp.pad(a, ((0, 0), (0, Mp - M), (0, 0), (0, 0), (0, 0)))
        return a.reshape(B, nb, span, dil, H, Dh)

    def with_prev(a):
        prev = jnp.pad(a, ((0, 0), (1, 0), (0, 0), (0, 0), (0, 0), (0, 0)))[:, :-1]
        return jnp.concatenate([prev, a], axis=2)

    qb = blocks(q)
    kk = with_prev(blocks(k))
    vv = with_prev(blocks(v))
    s = jnp.einsum('bnqrhc,bnkrhc->bnrhqk', qb.astype(jnp.float32), kk.astype(jnp.float32))
    s = s * (1.0 / math.sqrt(Dh))
    qpos = jnp.arange(span)[:, None]
    kpos = jnp.arange(2 * span)[None, :]
    dist = span + qpos - kpos
    band = (dist >= 0) & (dist <= span)
    not_first = (jnp.arange(nb) > 0)[:, None, None] | (kpos >= span)[None]
    valid = band[None] & not_first
    s = jnp.where(valid[None, :, None, None], s, -jnp.inf)
    mx = jnp.max(s, axis=-1, keepdims=True)
    p = jnp.exp(s - mx)
    den = jnp.sum(p, axis=-1)
    o = jnp.einsum('bnrhqk,bnkrhc->bnqrhc', p, vv.astype(jnp.float32))
    o = o / jnp.transpose(den, (0, 1, 4, 2, 3))[..., None]
    lse = jnp.transpose(mx[..., 0] + jnp.log(den), (0, 1, 4, 2, 3))
    o = o.reshape(B, Mp, dil, H, Dh)[:, :M].reshape(B, S, H, Dh)
    lse = lse.reshape(B, Mp, dil, H)[:, :M].reshape(B, S, H)
    return o, lse


def dilated_attention(q, k, v):
    B, S, H, Dh = q.shape
    outs, lses = [], []
    for window, dil in DILATED_PAIRS:
        o, lse = dilated_branch(q, k, v, dil, window // dil)
        outs.append(o)
        lses.append(lse)
    w = jax.nn.softmax(jnp.stack(lses, axis=0), axis=0)
    o = jnp.sum(w[..., None] * jnp.stack(outs, axis=0), axis=0)
    return o.reshape(B, S, H * Dh).astype(q.dtype)


def s5_mixer(u, lam_re, lam_im, log_dt, b_re, b_im, c_re, c_im, d_skip, w_glu):
    B, S, _ = u.shape
    f32 = jnp.float32
    uf = u.astype(f32).reshape(B, S, SSM_GROUPS, SSM_GROUP_CH)
    lam = lax.complex(lam_re.astype(f32), lam_im.astype(f32))
    dt = jnp.exp(log_dt.astype(f32))[:, None]
    lam_bar = jnp.exp(lam * dt)
    b_t = lax.complex(b_re.astype(f32), b_im.astype(f32))
    b_bar = ((lam_bar - 1.0) / lam)[..., None] * b_t
    c_t = lax.complex(c_re.astype(f32), c_im.astype(f32))
    bu = jnp.einsum('gph,bsgh->sbgp', b_bar, uf.astype(jnp.complex64))
    a = jnp.broadcast_to(lam_bar[None, None], (S, 1) + lam_bar.shape)

    def combine(left, right):
        a_l, b_l = left
        a_r, b_r = right
        return a_r * a_l, a_r * b_l + b_r

    _, states = lax.associative_scan(combine, (a, bu), axis=0)
    y = jnp.einsum('ghp,sbgp->bsgh', c_t, states).real + d_skip.astype(f32) * uf
    y = jax.nn.gelu(y.reshape(B, S, SSM_WIDTH))
    y = y * jax.nn.sigmoid(y @ w_glu.astype(f32))
    return y.astype(u.dtype)


def hier_moe(h, w_rg, b_rg, w_re, b_re_, w_gate, w_up, w_down):
    B, S, D = h.shape
    t = h.reshape(-1, D)
    grp_prob = jax.nn.softmax((t @ w_rg + b_rg).astype(jnp.float32), axis=-1)
    grp = jnp.argmax(grp_prob, axis=-1)
    g1 = jnp.take_along_axis(grp_prob, grp[:, None], axis=-1)
    exp_logits = (jnp.einsum('td,gde->tge', t, w_re) + b_re_).astype(jnp.float32)
    sel = jnp.take_along_axis(exp_logits, grp[:, None, None], axis=1)[:, 0]
    top_v, top_i = lax.top_k(sel, TOP_K_INNER)
    w2 = jax.nn.softmax(top_v, axis=-1) * g1
    expert_idx = grp[:, None] * EXPERTS_PER_GROUP + top_i
    gates = jnp.sum(jax.nn.one_hot(expert_idx, N_EXPERTS, dtype=jnp.float32) * w2[..., None], axis=1)
    hg = jnp.einsum('td,edf->tef', t, w_gate)
    hu = jnp.einsum('td,edf->tef', t, w_up)
    act = jax.nn.silu(hg) * hu * gates.astype(t.dtype)[..., None]
    out = jnp.einsum('tef,efd->td', act, w_down)
    return out.reshape(B, S, D)


def setup_inputs(seed: int = 0) -> dict:
    key = jax.random.key(seed)
    ks = jax.random.split(key, 24)
    nrm = jax.random.normal
    L, D = DEPTH, D_MODEL
    G, P, H = SSM_GROUPS, SSM_STATE, SSM_GROUP_CH
    return {
        "x": nrm(ks[0], (BATCH, SEQ, D), jnp.float32),
        "ln1_g": 1.0 + 0.02 * nrm(ks[1], (L, D), jnp.float32),
        "w_in": nrm(ks[2], (L, D, IN_WIDTH), jnp.float32) * D ** -0.5,
        "lam_re": -0.5 + 0.01 * nrm(ks[3], (L, G, P), jnp.float32),
        "lam_im": jnp.pi * jnp.arange(P, dtype=jnp.float32) + 0.01 * nrm(ks[4], (L, G, P), jnp.float32),
        "log_dt": jax.random.uniform(ks[5], (L, G), jnp.float32, math.log(DT_MIN), math.log(DT_MAX)),
        "b_re": nrm(ks[6], (L, G, P, H), jnp.float32) * (2 * H) ** -0.5,
        "b_im": nrm(ks[7], (L, G, P, H), jnp.float32) * (2 * H) ** -0.5,
        "c_re": nrm(ks[8], (L, G, H, P), jnp.float32) * (2 * P) ** -0.5,
        "c_im": nrm(ks[9], (L, G, H, P), jnp.float32) * (2 * P) ** -0.5,
        "d_skip": nrm(ks[10], (L, G, H), jnp.float32),
        "w_glu": nrm(ks[11], (L, SSM_WIDTH, SSM_WIDTH), jnp.float32) * SSM_WIDTH ** -0.5,
        "gn_attn": 1.0 + 0.02 * nrm(ks[12], (L, ATTN_WIDTH), jnp.float32),
        "gn_ssm": 1.0 + 0.02 * nrm(ks[13], (L, SSM_WIDTH), jnp.float32),
        "w_out": nrm(ks[14], (L, ATTN_WIDTH + SSM_WIDTH, D), jnp.float32) * (ATTN_WIDTH + SSM_WIDTH) ** -0.5,
        "ln2_g": 1.0 + 0.02 * nrm(ks[15], (L, D), jnp.float32),
        "w_router_grp": nrm(ks[16], (L, D, MOE_GROUPS), jnp.float32) * D ** -0.5,
        "b_router_grp": 0.01 * nrm(ks[17], (L, MOE_GROUPS), jnp.float32),
        "w_router_exp": nrm(ks[18], (L, MOE_GROUPS, D, EXPERTS_PER_GROUP), jnp.float32) * D ** -0.5,
        "b_router_exp": 0.01 * nrm(ks[19], (L, MOE_GROUPS, EXPERTS_PER_GROUP), jnp.float32),
        "w_gate": nrm(ks[20], (L, N_EXPERTS, D, EXPERT_FF), jnp.float32) * D ** -0.5,
        "w_up": nrm(ks[21], (L, N_EXPERTS, D, EXPERT_FF), jnp.float32) * D ** -0.5,
        "w_down": nrm(ks[22], (L, N_EXPERTS, EXPERT_FF, D), jnp.float32) * EXPERT_FF ** -0.5,
        "final_g": 1.0 + 0.02 * nrm(ks[23], (D,), jnp.float32),
    }


def reference(x, ln1_g, w_in, lam_re, lam_im, log_dt, b_re, b_im, c_re, c_im, d_skip, w_glu,
              gn_attn, gn_ssm, w_out, ln2_g, w_router_grp, b_router_grp, w_router_exp,
              b_router_exp, w_gate, w_up, w_down, final_g):
    B, S, D = x.shape
    for l in range(DEPTH):
        h = rmsnorm(x, ln1_g[l])
        proj = h @ w_in[l]
        q = proj[..., :ATTN_WIDTH].reshape(B, S, ATTN_HEADS, HEAD_DIM)
        k = proj[..., ATTN_WIDTH:2 * ATTN_WIDTH].reshape(B, S, ATTN_HEADS, HEAD_DIM)
        v = proj[..., 2 * ATTN_WIDTH:3 * ATTN_WIDTH].reshape(B, S, ATTN_HEADS, HEAD_DIM)
        u = proj[..., 3 * ATTN_WIDTH:]
        attn_out = dilated_attention(q, k, v)
        ssm_out = s5_mixer(u, lam_re[l], lam_im[l], log_dt[l], b_re[l], b_im[l],
                           c_re[l], c_im[l], d_skip[l], w_glu[l])
        mixed = jnp.concatenate([rmsnorm(attn_out, gn_attn[l]), rmsnorm(ssm_out, gn_ssm[l])], axis=-1)
        x = x + mixed @ w_out[l]
        h = rmsnorm(x, ln2_g[l])
        x = x + hier_moe(h, w_router_grp[l], b_router_grp[l], w_router_exp[l], b_router_exp[l],
                         w_gate[l], w_up[l], w_down[l])
    return rmsnorm(x, final_g)
```

```python
import math
import numpy as np
import concourse.bass as bass
import concourse.mybir as mybir
from concourse.bass_utils import run_bass_kernel_spmd
from contextlib import ExitStack

F32 = mybir.dt.float32
BF16 = mybir.dt.bfloat16
I32 = mybir.dt.int32
ALU = mybir.AluOpType
AF = mybir.ActivationFunctionType
AX = mybir.AxisListType

NDMA_SLOTS = 6
DEPTH = 4
S_LEN = 2048
D = 1024
NT = 16
LC = 128
NCH = S_LEN // LC
TWO_PI = 2.0 * math.pi


class _Op:
    __slots__ = ("eng", "fn", "reads", "writes", "deps", "idx", "sig", "is_dma",
                 "slot", "val", "cnt")


class Sched:
    ENGS = ("pe", "act", "dve", "pool", "sp")

    def __init__(self, same_engine_sync=True):
        self.ops = []
        self.last_w = {}
        self.readers = {}
        self.same_engine_sync = same_engine_sync
        self.since_barrier = []

    def add(self, eng, fn, reads=(), writes=(), dma=False):
        op = _Op()
        op.eng = eng
        op.fn = fn
        op.reads = tuple(reads)
        op.writes = tuple(writes)
        op.is_dma = dma
        op.idx = len(self.ops)
        op.sig = False
        deps = set()
        for r in op.reads:
            w = self.last_w.get(r)
            if w is not None:
                deps.add(w)
        for w_ in op.writes:
            w = self.last_w.get(w_)
            if w is not None:
                deps.add(w)
            for rd in self.readers.get(w_, ()):
                deps.add(rd)
        deps.discard(op)
        op.deps = deps
        for r in op.reads:
            self.readers.setdefault(r, []).append(op)
        for w_ in op.writes:
            self.last_w[w_] = op
            self.readers[w_] = []
        self.ops.append(op)
        self.since_barrier.append(op)
        return op

    def barrier(self):
        last = {}
        dmas = []
        for op in self.since_barrier:
            if op.is_dma:
                dmas.append(op)
            elif op.fn is not None:
                last[op.eng] = op
        deps = set(last.values()) | set(dmas)
        self.since_barrier = []
        for e in self.ENGS:
            op = _Op()
            op.eng = e
            op.fn = None
            op.reads = ()
            op.writes = ()
            op.is_dma = False
            op.idx = len(self.ops)
            op.sig = False
            op.deps = set(d for d in deps)
            self.ops.append(op)

    def finalize(self):
        for op in self.ops:
            for d in op.deps:
                if d.is_dma:
                    d.sig = True
                elif d.eng == op.eng and not op.is_dma:
                    if op.eng == "pe":
                        continue
                    if self.same_engine_sync or op.fn is None:
                        d.sig = True
                else:
                    d.sig = True
        cnt = {e: 0 for e in self.ENGS}
        dcnt = {e: 0 for e in self.ENGS}
        for op in self.ops:
            if op.is_dma:
                j = dcnt[op.eng]
                dcnt[op.eng] += 1
                op.slot = j % NDMA_SLOTS
                op.val = 16 * (j // NDMA_SLOTS + 1)
                op.cnt = j
            elif op.sig:
                cnt[op.eng] += 1
                op.val = cnt[op.eng]

    def emit(self, nc, es):
        self.finalize()
        sems = {e: es.enter_context(nc.semaphore("s_" + e)) for e in self.ENGS}
        dsems = {}
        for e in ("sp", "act", "pool"):
            dsems[e] = [es.enter_context(nc.semaphore("d_%s%d" % (e, i))) for i in range(NDMA_SLOTS)]
        block = es.enter_context(nc.Block())
        per = {e: [op for op in self.ops if op.eng == e] for e in self.ENGS}

        def run(engname, eng):
            seen = {}

            def wait(key, sem, val):
                if seen.get(key, 0) >= val:
                    return
                seen[key] = val
                if getattr(self, "trace", None) is not None:
                    self.trace.append((engname, "wait", key, val))
                eng.wait_ge(sem, val)

            for op in per[engname]:
                need = {}
                for d in op.deps:
                    if d.is_dma:
                        key = ("d", d.eng, d.slot)
                        need[key] = max(need.get(key, 0), d.val)
                    else:
                        if d.eng == engname and not op.is_dma:
                            if engname == "pe" or not (self.same_engine_sync or op.fn is None):
                                continue
                        if not d.sig:
                            continue
                        key = ("e", d.eng)
                        need[key] = max(need.get(key, 0), d.val)
                if op.is_dma and op.cnt >= NDMA_SLOTS:
                    key = ("d", engname, op.slot)
                    need[key] = max(need.get(key, 0), op.val - 16)
                for key, val in need.items():
                    if key[0] == "d":
                        wait(key, dsems[key[1]][key[2]], val)
                    else:
                        wait(key, sems[key[1]], val)
                if getattr(self, "trace", None) is not None:
                    self.trace.append((engname, "op", op.idx, op.is_dma, getattr(op, "slot", None), getattr(op, "val", None), op.sig, op.writes))
                if op.fn is None:
                    continue
                inst = op.fn(eng)
                if op.is_dma:
                    inst.then_inc(dsems[engname][op.slot], 16)
                elif op.sig:
                    inst.then_inc(sems[engname], 1)
            lastd = {}
            for op in per[engname]:
                if op.is_dma:
                    lastd[op.slot] = op.val
            for slot, val in lastd.items():
                eng.wait_ge(dsems[engname][slot], val)

        @block.tensor
        def _(e):
            run("pe", e)

        @block.scalar
        def _(e):
            run("act", e)

        @block.vector
        def _(e):
            run("dve", e)

        @block.gpsimd
        def _(e):
            run("pool", e)

        @block.sync
        def _(e):
            run("sp", e)


class _Stop(Exception):
    pass


def build(depth=DEPTH, dbg=False, stop=99):
    nc = bass.Bass("TRN2", target_bir_lowering=False)
    L = depth

    def din(name, shape):
        return nc.dram_tensor(name, list(shape), F32, kind="ExternalInput").ap()

    x_d = din("x", [S_LEN, D])
    w_in_d = din("w_in", [L, D, 2048])
    w_out_d = din("w_out", [L, D, D])
    w_glu_d = din("w_glu", [L, 512, 512])
    w_gu_d = din("w_gu", [L, 16, D, 512])
    w_dn_d = din("w_dn", [L, 16, 256, D])
    w_r_d = din("w_r", [L, D, 20])
    b_r_d = din("b_r", [L, 1, 20])
    ln1_d = din("ln1", [L, 128, 8])
    ln2_d = din("ln2", [L, 128, 8])
    gna_d = din("gna", [L, 128, 4])
    gns_d = din("gns", [L, 128, 4])
    fin_d = din("fin", [1, D])
    lamre_d = din("lamre", [L, 128, 16])
    lamim_d = din("lamim", [L, 128, 16])
    logdt_d = din("logdt", [L, 128, 16])
    bre_d = din("bre", [L, 128, 16, 32])
    bim_d = din("bim", [L, 128, 16, 32])
    cre_d = din("cre", [L, 128, 16, 32])
    cim_d = din("cim", [L, 128, 16, 32])
    dsk_d = din("dsk", [L, 128, 4])
    out_d = nc.dram_tensor("out", [S_LEN, D], F32, kind="ExternalOutput").ap()
    scr_d = nc.dram_tensor("scr", [2, S_LEN, 520], F32, kind="Internal").ap()
    dbg_d = {}
    if dbg:
        dbg_d["x1"] = nc.dram_tensor("dbg_x1", [S_LEN, D], F32, kind="ExternalOutput").ap()
        dbg_d["mixA"] = nc.dram_tensor("dbg_mixA", [128, 4, S_LEN], F32, kind="ExternalOutput").ap()
        dbg_d["mixS"] = nc.dram_tensor("dbg_mixS", [128, 4, S_LEN], F32, kind="ExternalOutput").ap()
        dbg_d["gates"] = nc.dram_tensor("dbg_gates", [128, NT, 16], F32, kind="ExternalOutput").ap()
        dbg_d["qT"] = nc.dram_tensor("dbg_qT", [128, 4, S_LEN], F32, kind="ExternalOutput").ap()
        dbg_d["uT"] = nc.dram_tensor("dbg_uT", [128, 4, S_LEN], F32, kind="ExternalOutput").ap()
        dbg_d["rs"] = nc.dram_tensor("dbg_rs", [128, NT], F32, kind="ExternalOutput").ap()

    S = Sched()
    if dbg:
        S.trace = []
        build.last_sched = S
    add = S.add
    with ExitStack() as es:
        def sb(name, shape, dt):
            return es.enter_context(nc.sbuf_tensor("sb_" + name, list(shape), dt))

        ARENA_W = 48 * 1024
        arena = sb("arena", [128, ARENA_W], F32)

        def view(off_kib, shape, dt):
            nwords_per = 1 if dt in (F32, I32) else 0.5
            n = int(np.prod(shape[1:]) * nwords_per)
            off = int(off_kib * 256)
            assert off + n <= ARENA_W, (off_kib, shape)
            a = arena[:, off:off + n]
            if dt != F32:
                a = a.bitcast(dt)
            if len(shape) > 2:
                names = ["d%d" % i for i in range(len(shape) - 1)]
                pat = "p (" + " ".join(names) + ") -> p " + " ".join(names)
                kw = {names[i]: int(shape[1 + i]) for i in range(len(names) - 1)}
                a = a.rearrange(pat, **kw)
            return a

        x = view(0, [128, NT, D], F32)
        ident = sb("ident", [128, 128], BF16)
        identf = sb("identf", [128, 128], F32)
        mask = sb("mask", [128, 256], BF16)
        ones_b = sb("ones_b", [128, 1], BF16)
        iota_t = sb("iota_t", [128, LC + 1], F32)
        eps_t = sb("eps_t", [128, 1], F32)
        halfpi = sb("halfpi", [128, 1], F32)
        ln1 = sb("ln1", [128, 8], F32)
        ln2 = sb("ln2", [128, 8], F32)
        gna = sb("gna", [128, 4], F32)
        gns = sb("gns", [128, 4], F32)
        dsk = sb("dsk", [128, 4], F32)
        b_r = sb("b_r", [128, 20], F32)
        w_r = sb("w_r", [128, 8, 20], F32)
        gates = sb("gates", [128, NT, 16], F32)
        rstd_s = sb("rstd_s", [128, NT], F32)
        sm = sb("sm", [128, 16, 32], F32)
        junk = sb("junk", [128, D], BF16)
        nrm = sb("nrm", [128, 32], F32)
        lall = sb("lall", [128, NT, 20], F32)
        rt = sb("rt", [128, 960], F32)
        maskf = rt[:, 0:256]
        onb_p = sb("onb_p", [128, 512], BF16)
        ps = [es.enter_context(nc.psum_tensor("ps%d" % i, [128, 512], F32)) for i in range(8)]

        def PS(i):
            return ("ps", i)

        add("pool", lambda e: e.memset(identf[:], 0.0), writes=["identf"])
        add("pool", lambda e: e.affine_select(out=identf[:], in_=identf[:], pattern=[[-1, 128]],
                                              compare_op=ALU.not_equal, fill=1.0, base=0, channel_multiplier=1),
            reads=["identf"], writes=["identf"])
        add("pool", lambda e: e.tensor_copy(out=ident[:], in_=identf[:]), reads=["identf"], writes=["ident"])
        add("pool", lambda e: e.memset(maskf, 1.0), writes=["maskf"])
        add("pool", lambda e: e.affine_select(out=maskf[:, 0:128], in_=maskf[:, 0:128], pattern=[[1, 128]],
                                              compare_op=ALU.is_ge, fill=0.0, base=0, channel_multiplier=-1),
            reads=["maskf"], writes=["maskf"])
        add("pool", lambda e: e.affine_select(out=maskf[:, 128:256], in_=maskf[:, 128:256], pattern=[[-1, 128]],
                                              compare_op=ALU.is_ge, fill=0.0, base=0, channel_multiplier=1),
            reads=["maskf"], writes=["maskf"])
        add("pool", lambda e: e.tensor_scalar(out=mask[:], in0=maskf, scalar1=-1.0, scalar2=30000.0, op0=ALU.add, op1=ALU.mult), reads=["maskf"], writes=["mask"])
        add("pool", lambda e: e.memset(ones_b[:], 1.0), writes=["ones_b"])
        add("pool", lambda e: e.memset(eps_t[:], 1e-6), writes=["eps"])
        add("pool", lambda e: e.memset(halfpi[:], math.pi / 2), writes=["halfpi"])
        add("pool", lambda e: e.iota(iota_t[:], pattern=[[1, LC + 1]], base=0, channel_multiplier=0,
                                     allow_small_or_imprecise_dtypes=True), writes=["iota"])
        for t in range(NT):
            add("sp", lambda e, t=t: e.dma_start(out=x[:, t, :], in_=x_d[t * 128:(t + 1) * 128, :]),
                writes=[("x", t)], dma=True)

        def emit_sq(t):
            add("act", lambda e, t=t: e.activation(out=junk[:], in_=x[:, t, :], func=AF.Square, accum_out=nrm[:, t:t + 1]),
                reads=[("x", t)], writes=["junk", ("ss", t)])

        def rmsnorm_T(hT, g_sb, gname, router, squares_done=False):
            xs2 = [view(160, [128, D], F32), view(164, [128, D], F32)]
            h32 = view(168, [128, 8, 128], F32)
            ssall = nrm[:, 0:16]
            rsall = nrm[:, 16:32]
            if not squares_done:
                for t in range(NT):
                    emit_sq(t)
            add("act", lambda e: e.activation(out=rsall, in_=ssall, func=AF.Sqrt, scale=1.0 / D, bias=eps_t[:]),
                reads=[("ss", t) for t in range(NT)] + ["eps"], writes=["rs0", "rs"])
            add("dve", lambda e: e.reciprocal(out=rsall, in_=rsall), reads=["rs0"], writes=["rs"])
            xsb2 = [view(160, [128, D], BF16), view(164, [128, D], BF16)]

            def st_S(t):
                xs = xs2[t % 2] if router else xsb2[t % 2]
                add("dve", lambda e, t=t, xs=xs: e.tensor_scalar(out=xs, in0=x[:, t, :], scalar1=rsall[:, t:t + 1], scalar2=None, op0=ALU.mult),
                    reads=[("x", t), "rs"], writes=[("xs", t % 2)])

            def st_X(t):
                xs = xs2[t % 2]
                b0 = 2 * (t % 2)
                if not router:
                    xsb = xsb2[t % 2]
                    pbf = ps[b0][:].bitcast(BF16)
                    def f_trb(e, xsb=xsb, pbf=pbf):
                        last = None
                        for k in range(8):
                            last = e.transpose(out=pbf[:, k * 128:(k + 1) * 128], in_=xsb[:, k * 128:(k + 1) * 128], identity=ident[:])
                        return last
                    add("pe", f_trb, reads=[("xs", t % 2), "ident"], writes=[PS(b0)])
                    return
                def f_tr(e, xs=xs, b0=b0):
                    last = None
                    for k in range(8):
                        last = e.transpose(out=ps[b0 + k // 4][:, (k % 4) * 128:(k % 4 + 1) * 128], in_=xs[:, k * 128:(k + 1) * 128],
                                           identity=identf[:])
                    return last
                add("pe", f_tr, reads=[("xs", t % 2), "identf"], writes=[PS(b0), PS(b0 + 1)])

            def st_E(t):
                b0 = 2 * (t % 2)
                for hlf in range(2):
                    if router:
                        add("dve", lambda e, hlf=hlf, b0=b0: e.tensor_tensor(
                            out=h32[:, 4 * hlf:4 * hlf + 4, :], in0=ps[b0 + hlf][:].rearrange("p (a b) -> p a b", a=4),
                            in1=g_sb[:, 4 * hlf:4 * hlf + 4].unsqueeze(2).to_broadcast([128, 4, 128]), op=ALU.mult),
                            reads=[PS(b0 + hlf), gname], writes=[("h32", hlf)])
                        add("act", lambda e, hlf=hlf, t=t: e.activation(out=hT[:, 4 * hlf:4 * hlf + 4, t * 128:(t + 1) * 128],
                                                                       in_=h32[:, 4 * hlf:4 * hlf + 4, :], func=AF.Copy),
                            reads=[("h32", hlf)], writes=[("hT", t)])
                    elif hlf == 0:
                        add("dve", lambda e, t=t, b0=b0: e.tensor_tensor(
                            out=hT[:, :, t * 128:(t + 1) * 128], in0=ps[b0][:].bitcast(BF16).rearrange("p (a b) -> p a b", a=8),
                            in1=g_sb[:].unsqueeze(2).to_broadcast([128, 8, 128]), op=ALU.mult),
                            reads=[PS(b0), gname], writes=[("hT", t)])
                if router:
                    def f_mm(e, t=t):
                        last = None
                        for k in range(8):
                            last = e.matmul(ps[4 + t % 2][:, 0:20], lhsT=h32[:, k, :], rhs=w_r[:, k, :], start=(k == 0), stop=(k == 7))
                        return last
                    add("pe", f_mm, reads=[("h32", 0), ("h32", 1), "w_r"], writes=[PS(4 + t % 2)])
                    add("dve", lambda e, t=t: e.tensor_tensor(out=lall[:, t, :], in0=ps[4 + t % 2][:, 0:20], in1=b_r[:], op=ALU.add),
                        reads=[PS(4 + t % 2), "b_r"], writes=[("lall", t)])

            st_S(0)
            st_X(0)
            for t in range(NT):
                if t + 1 < NT:
                    st_S(t + 1)
                    st_X(t + 1)
                st_E(t)
            if router:
                router_batched()

        def router_batched():
            T_ = NT
            lall_r = [("lall", t) for t in range(NT)]
            lg = lall[:, :, 0:4]
            le = lall[:, :, 4:20].rearrange("p t (g e) -> p t g e", g=4)
            _off = [0]
            def V(k):
                o = _off[0]
                _off[0] += 16 * k
                return rt[:, o:o + 16 * k].rearrange("p (t k) -> p t k", t=16)
            m = V(1); ohg = V(4); eg = V(4); sg = V(1); g1 = V(1)
            sel = V(4); v1 = V(1); oh1 = V(4); sel2 = V(4); v2 = V(1); oh2 = V(4)
            dv = V(1); ex = V(1); w1 = V(1); w2 = V(1); inner = V(4); inner2 = V(4)
            t16 = V(16).rearrange("p t (g e) -> p t g e", g=4)
            b4 = lambda ap_: ap_.to_broadcast([128, 16, 4])
            add("dve", lambda e: e.tensor_reduce(out=m, in_=lg, axis=AX.X, op=ALU.max), reads=lall_r, writes=["r_m"])
            add("dve", lambda e: e.tensor_tensor(out=ohg, in0=lg, in1=b4(m), op=ALU.is_equal), reads=lall_r + ["r_m"], writes=["r_ohg"])
            add("dve", lambda e: e.tensor_tensor(out=eg, in0=lg, in1=b4(m), op=ALU.subtract), reads=lall_r + ["r_m"], writes=["r_eg0"])
            add("act", lambda e: e.activation(out=eg, in_=eg, func=AF.Exp), reads=["r_eg0"], writes=["r_eg"])
            add("dve", lambda e: e.tensor_reduce(out=sg, in_=eg, axis=AX.X, op=ALU.add), reads=["r_eg"], writes=["r_sg"])
            add("dve", lambda e: e.reciprocal(out=g1, in_=sg), reads=["r_sg"], writes=["r_g1"])
            add("dve", lambda e: e.tensor_tensor(out=t16, in0=le, in1=ohg.unsqueeze(3).to_broadcast([128, 16, 4, 4]), op=ALU.mult),
                reads=lall_r + ["r_ohg"], writes=["r_t16"])
            add("dve", lambda e: e.tensor_reduce(out=sel, in_=t16.rearrange("p t g e -> p t e g"), axis=AX.X, op=ALU.add),
                reads=["r_t16"], writes=["r_sel"])
            add("dve", lambda e: e.tensor_reduce(out=v1, in_=sel, axis=AX.X, op=ALU.max), reads=["r_sel"], writes=["r_v1"])
            add("dve", lambda e: e.tensor_tensor(out=oh1, in0=sel, in1=b4(v1), op=ALU.is_equal), reads=["r_sel", "r_v1"], writes=["r_oh1"])
            add("dve", lambda e: e.scalar_tensor_tensor(out=sel2, in0=oh1, scalar=-1e30, in1=sel, op0=ALU.mult, op1=ALU.add),
                reads=["r_oh1", "r_sel"], writes=["r_sel2"])
            add("dve", lambda e: e.tensor_reduce(out=v2, in_=sel2, axis=AX.X, op=ALU.max), reads=["r_sel2"], writes=["r_v2"])
            add("dve", lambda e: e.tensor_tensor(out=oh2, in0=sel2, in1=b4(v2), op=ALU.is_equal), reads=["r_sel2", "r_v2"], writes=["r_oh2"])
            add("dve", lambda e: e.tensor_tensor(out=dv, in0=v2, in1=v1, op=ALU.subtract), reads=["r_v1", "r_v2"], writes=["r_dv"])
            add("act", lambda e: e.activation(out=ex, in_=dv, func=AF.Exp), reads=["r_dv"], writes=["r_ex"])
            add("dve", lambda e: e.tensor_scalar(out=ex, in0=ex, scalar1=1.0, scalar2=None, op0=ALU.add), reads=["r_ex"], writes=["r_den"])
            add("dve", lambda e: e.reciprocal(out=ex, in_=ex), reads=["r_den"], writes=["r_rden"])
            add("dve", lambda e: e.tensor_tensor(out=w1, in0=ex, in1=g1, op=ALU.mult), reads=["r_rden", "r_g1"], writes=["r_w1"])
            add("dve", lambda e: e.tensor_tensor(out=w2, in0=g1, in1=w1, op=ALU.subtract), reads=["r_w1", "r_g1"], writes=["r_w2"])
            add("dve", lambda e: e.tensor_tensor(out=inner, in0=oh1, in1=b4(w1), op=ALU.mult), reads=["r_oh1", "r_w1"], writes=["r_in0"])
            add("dve", lambda e: e.tensor_tensor(out=inner2, in0=oh2, in1=b4(w2), op=ALU.mult), reads=["r_oh2", "r_w2"], writes=["r_in1"])
            add("dve", lambda e: e.tensor_tensor(out=inner, in0=inner, in1=inner2, op=ALU.add), reads=["r_in0", "r_in1"], writes=["r_in"])
            add("dve", lambda e: e.tensor_tensor(out=gates[:].rearrange("p t (g e) -> p t g e", g=4),
                                                 in0=ohg.unsqueeze(3).to_broadcast([128, 16, 4, 4]),
                                                 in1=inner.unsqueeze(2).to_broadcast([128, 16, 4, 4]), op=ALU.mult),
                reads=["r_ohg", "r_in"], writes=[("gates", t) for t in range(NT)])

        def load_w_cast(dst, src, tok):
            add("pool", lambda e: e.dma_start(out=dst, in_=src), writes=[tok], dma=True)

        for l in range(L):
          try:
            for (dst, src, nm) in ((ln1, ln1_d, "ln1"), (ln2, ln2_d, "ln2"), (gna, gna_d, "gna"), (gns, gns_d, "gns"),
                                   (dsk, dsk_d, "dsk")):
                add("sp", lambda e, dst=dst, src=src, l=l: e.dma_start(out=dst[:], in_=src[l]), writes=[nm], dma=True)
            add("sp", lambda e, l=l: e.dma_start(out=b_r[:], in_=b_r_d[l].partition_broadcast(128)), writes=["b_r"], dma=True)
            add("sp", lambda e, l=l: e.dma_start(out=w_r[:], in_=w_r_d[l].rearrange("(k p) n -> p k n", p=128)), writes=["w_r"], dma=True)

            hT = view(64, [128, 8, S_LEN], BF16)
            wsl = [view(96, [128, 8, 512], BF16), view(104, [128, 8, 512], BF16)]
            qT = view(112, [128, 4, S_LEN], BF16)
            kT = view(128, [128, 4, S_LEN], BF16)
            vT = view(144, [128, 4, S_LEN], BF16)
            uT = view(160, [128, 4, S_LEN], BF16)
            w_in_v = w_in_d[l].rearrange("(k p) n -> p k n", p=128)
            for c in range(2):
                load_w_cast(wsl[c][:], w_in_v[:, :, c * 512:(c + 1) * 512], ("wsl", c))
            rmsnorm_T(hT, ln1, "ln1", router=False, squares_done=(l > 0))
            S.barrier()
            if stop == 1:
                raise _Stop()
            dsts = [qT, kT, vT, uT]
            names = ["qT", "kT", "vT", "uT"]
            for c in range(4):
                for fc in range(4):
                    for tb in range(4):
                        bank = (fc * 4 + tb) % 4
                        def f_mm(e, c=c, fc=fc, tb=tb, bank=bank):
                            last = None
                            for k in range(8):
                                last = e.matmul(ps[bank][:], lhsT=wsl[c % 2][:, k, fc * 128:(fc + 1) * 128],
                                                rhs=hT[:, k, tb * 512:(tb + 1) * 512], start=(k == 0), stop=(k == 7))
                            return last
                        add("pe", f_mm, reads=[("wsl", c % 2)] + [("hT", t) for t in range(4 * tb, 4 * tb + 4)], writes=[PS(bank)])
                        eng = "act" if (tb % 2 == 0) else "dve"
                        if eng == "act":
                            add("act", lambda e, c=c, fc=fc, tb=tb, bank=bank: e.activation(
                                out=dsts[c][:, fc, tb * 512:(tb + 1) * 512], in_=ps[bank][:], func=AF.Copy),
                                reads=[PS(bank)], writes=[(names[c], fc, tb)])
                        else:
                            add("dve", lambda e, c=c, fc=fc, tb=tb, bank=bank: e.tensor_copy(
                                out=dsts[c][:, fc, tb * 512:(tb + 1) * 512], in_=ps[bank][:]),
                                reads=[PS(bank)], writes=[(names[c], fc, tb)])
                if c + 2 < 4:
                    load_w_cast(wsl[c % 2][:], w_in_v[:, :, (c + 2) * 512:(c + 3) * 512], ("wsl", c % 2))
            S.barrier()
            if dbg and l == 0:
                dq = view(64, [128, 4, S_LEN], F32)
                for (srcT, nm) in ((qT, "qT"), (uT, "uT")):
                    add("dve", lambda e, srcT=srcT, dq=dq: e.tensor_copy(out=dq, in_=srcT), writes=["dq"])
                    for cq in range(4):
                        add("sp", lambda e, nm=nm, cq=cq, dq=dq: e.dma_start(out=dbg_d[nm][:, cq, :], in_=dq[:, cq, :]), reads=["dq"], dma=True)
                    S.barrier()

            if stop == 2:
                raise _Stop()
            mixA = view(64, [128, 4, S_LEN], BF16)
            Vb = [view(80 + 1.25 * i, [128, 8, 65], BF16) for i in range(4)]
            pT = [view(85 + 4 * i, [128, 4, 2, 256], BF16) for i in range(4)]
            ost = [view(101 + 2.25 * i, [128, 8, 65], F32) for i in range(2)]
            cmb = [view(105.5 + 2.25 * i, [128, 8, 65], F32) for i in range(2)]
            onrm = view(110, [128, 8, 64], F32)
            onb = onb_p
            for i in range(4):
                add("pool", lambda e, i=i: e.memset(Vb[i][:, :, 64:65], 1.0), writes=[("Vb", i)])
            blk_ctr = 0
            pending = None
            for bi, dil in enumerate((4, 16, 1)):
                nbc = 16 // dil
                for b in range(16):
                    r, n = b // nbc, b % nbc
                    t0 = 128 * n * dil + r
                    has_next = (n < nbc - 1)
                    has_prev = (n > 0)
                    nq = 256 if has_next else 128
                    vslot = blk_ctr % 4
                    ptr = ps[4][:].bitcast(BF16)
                    def f_vt(e, t0=t0, dil=dil):
                        last = None
                        for hp in range(4):
                            last = e.transpose(out=ptr[:, hp * 128:(hp + 1) * 128],
                                               in_=vT[:, hp, t0:t0 + 127 * dil + 1:dil], identity=ident[:])
                        return last
                    add("pe", f_vt, reads=[("vT", hp, tb) for hp in range(4) for tb in range(4)] + ["ident"], writes=[PS(4)])
                    add("act", lambda e, vslot=vslot: e.activation(
                        out=Vb[vslot][:, :, 0:64], in_=ptr[:, 0:512].rearrange("p (h c) -> p h c", h=8), func=AF.Copy),
                        reads=[PS(4)], writes=[("Vb", vslot)])
                    pslot = blk_ctr % 4
                    for hpp in range(2):
                        def f_sc(e, hpp=hpp, t0=t0, dil=dil, nq=nq):
                            last = None
                            for hpo in range(2):
                                hp = 2 * hpp + hpo
                                for hh in range(2):
                                    last = e.matmul(ps[2 * hpp + hh][:, hpo * 256:hpo * 256 + nq],
                                                    lhsT=kT[hh * 64:(hh + 1) * 64, hp, t0:t0 + 127 * dil + 1:dil],
                                                    rhs=qT[hh * 64:(hh + 1) * 64, hp, t0:t0 + (nq - 1) * dil + 1:dil], start=(hpo == 0), stop=False,
                                                    skip_group_check=True)
                            for hpo in range(2):
                                for hh in range(2):
                                    last = e.matmul(ps[2 * hpp + hh][:, hpo * 256:hpo * 256 + nq], lhsT=ident[:], rhs=mask[:, 0:nq],
                                                    start=False, stop=True, skip_group_check=True)
                            return last
                        add("pe", f_sc, reads=[("kT", hp, tb) for hp in (2 * hpp, 2 * hpp + 1) for tb in range(4)] +
                            [("qT", hp, tb) for hp in (2 * hpp, 2 * hpp + 1) for tb in range(4)] + ["mask", "ident"], writes=[PS(2 * hpp), PS(2 * hpp + 1)])
                        for hh in range(2):
                            bank = 2 * hpp + hh
                            add("act", lambda e, pslot=pslot, bank=bank, nq=nq, hpp=hpp, hh=hh: e.activation(
                                out=pT[pslot][:, 2 * hpp:2 * hpp + 2, hh, 0:nq], in_=ps[bank][:].rearrange("p (h q) -> p h q", h=2)[:, :, 0:nq],
                                func=AF.Exp, scale=0.125), reads=[PS(bank)], writes=[("pT", pslot, hpp, hh)])
                    pall = [("pT", pslot, hpp, hh) for hpp in range(2) for hh in range(2)]
                    pass
                    def back(blk_ctr=blk_ctr, has_prev=has_prev, vslot=vslot, dil=dil, bi=bi, b=b, t0=t0):
                        def f_pv(e, blk_ctr=blk_ctr, has_prev=has_prev, vslot=vslot):
                            last = None
                            for h in range(8):
                                hp, hh = h // 2, h % 2
                                bank = 5 + h // 4
                                col = (h % 4) * 65
                                first = True
                                if has_prev:
                                    pprev = (blk_ctr - 1) % 4
                                    vprev = (blk_ctr - 1) % 4
                                    last = e.matmul(ps[bank][:, col:col + 65], lhsT=pT[pprev][:, hp, hh, 128:256],
                                                    rhs=Vb[vprev][:, h, :], start=True, stop=False)
                                    first = False
                                pcur = blk_ctr % 4
                                last = e.matmul(ps[bank][:, col:col + 65], lhsT=pT[pcur][:, hp, hh, 0:128],
                                                rhs=Vb[vslot][:, h, :], start=first, stop=True)
                            return last
                        rd = [("pT", blk_ctr % 4, a_, b_) for a_ in range(2) for b_ in range(2)] + [("Vb", vslot)]
                        if has_prev:
                            rd += [("pT", (blk_ctr - 1) % 4, a_, b_) for a_ in range(2) for b_ in range(2)] + [("Vb", (blk_ctr - 1) % 4)]
                        add("pe", f_pv, reads=rd, writes=[PS(5), PS(6)])
                        oslot = blk_ctr % 2
                        add("act", lambda e, oslot=oslot: e.activation(
                            out=ost[oslot][:, 0:4, :], in_=ps[5][:, 0:260].rearrange("p (h c) -> p h c", h=4), func=AF.Copy),
                            reads=[PS(5)], writes=[("ost", oslot, 0)])
                        add("dve", lambda e, oslot=oslot: e.tensor_copy(
                            out=ost[oslot][:, 4:8, :], in_=ps[6][:, 0:260].rearrange("p (h c) -> p h c", h=4)),
                            reads=[PS(6)], writes=[("ost", oslot, 1)])
                        if dil != 1:
                            dst = scr_d[bi, t0:t0 + 127 * dil + 1:dil, :]
                            add("sp", lambda e, dst=dst, oslot=oslot: e.dma_start(out=dst, in_=ost[oslot][:].rearrange("p h c -> p (h c)")),
                                reads=[("ost", oslot, 0), ("ost", oslot, 1)], writes=[("scr", bi, b)], dma=True)
                        else:
                            for j in range(2):
                                add("sp", lambda e, j=j, b=b: e.dma_start(out=cmb[j][:].rearrange("p h c -> p (h c)"),
                                                                          in_=scr_d[j, b * 128:(b + 1) * 128, :]),
                                    reads=[("scr", j, bb) for bb in range(16)], writes=[("cmb", j)], dma=True)
                            add("dve", lambda e: e.tensor_tensor(out=cmb[0][:], in0=cmb[0][:], in1=cmb[1][:], op=ALU.add),
                                reads=[("cmb", 0), ("cmb", 1)], writes=[("cmb", 0)])
                            add("dve", lambda e, oslot=oslot: e.tensor_tensor(out=cmb[0][:], in0=cmb[0][:], in1=ost[oslot][:], op=ALU.add),
                                reads=[("cmb", 0), ("ost", oslot, 0), ("ost", oslot, 1)], writes=[("cmb", 0)])
                            rden = sm[:, 8, 0:8]
                            add("dve", lambda e: e.reciprocal(out=rden, in_=cmb[0][:, :, 64]), reads=[("cmb", 0)], writes=["rden"])
                            add("dve", lambda e: e.tensor_tensor(out=onrm[:], in0=cmb[0][:, :, 0:64],
                                                                 in1=rden.unsqueeze(2).to_broadcast([128, 8, 64]), op=ALU.mult),
                                reads=[("cmb", 0), "rden"], writes=["onrm"])
                            ssa = sm[:, 8, 8:9]
                            rsa = sm[:, 8, 9:10]
                            add("act", lambda e: e.activation(out=junk[:, 0:512], in_=onrm[:].rearrange("p h c -> p (h c)"),
                                                              func=AF.Square, accum_out=ssa), reads=["onrm"], writes=["junk", "ssa"])
                            add("act", lambda e: e.activation(out=rsa, in_=ssa, func=AF.Ln, scale=1.0 / 512, bias=eps_t[:]),
                                reads=["ssa", "eps"], writes=["rsa0", "rsa"])
                            add("act", lambda e: e.activation(out=rsa, in_=rsa, func=AF.Exp, scale=-0.5), reads=["rsa0"], writes=["rsa"])
                            add("act", lambda e: e.activation(out=onb[:], in_=onrm[:].rearrange("p h c -> p (h c)"), func=AF.Copy, scale=rsa),
                                reads=["onrm", "rsa"], writes=["onb"])
                            ptm = ps[7][:].bitcast(BF16)
                            def f_tm(e):
                                last = None
                                for k in range(4):
                                    last = e.transpose(out=ptm[:, k * 128:(k + 1) * 128], in_=onb[:, k * 128:(k + 1) * 128], identity=ident[:])
                                return last
                            add("pe", f_tm, reads=["onb", "ident"], writes=[PS(7)])
                            add("dve", lambda e, b=b: e.tensor_tensor(
                                out=mixA[:, :, b * 128:(b + 1) * 128], in0=ptm[:, 0:512].rearrange("p (k t) -> p k t", k=4),
                                in1=gna[:].unsqueeze(2).to_broadcast([128, 4, 128]), op=ALU.mult),
                                reads=[PS(7), "gna"], writes=[("mixA", b)])

                    if pending is not None:
                        pending()
                    pending = back
                    blk_ctr += 1
            pending()
            S.barrier()
            if dbg and l == 0:
                dq = view(112, [128, 4, S_LEN], F32)
                add("dve", lambda e, dq=dq: e.tensor_copy(out=dq, in_=mixA), writes=["dq"])
                for cq in range(4):
                    add("sp", lambda e, cq=cq, dq=dq: e.dma_start(out=dbg_d["mixA"][:, cq, :], in_=dq[:, cq, :]), reads=["dq"], dma=True)
                S.barrier()

            if stop == 3:
                raise _Stop()
            LB = 64
            NQ = 4
            NTB = 16 * (LB + 1)
            mixS = view(80, [128, 4, S_LEN], BF16)
            bufA = view(96, [128, 16, LB], F32)
            bufB = view(100, [128, 16, LB], F32)
            bufT1 = view(104, [128, 16, LB], F32)
            bufT2 = view(108, [128, 16, LB], F32)
            tcos = view(112, [128, 16, LB + 1], F32)
            tsin = view(116.25, [128, 16, LB + 1], F32)
            Sp_re = view(120.5, [128, 16, LB + 1], BF16)
            Sp_im = view(122.75, [128, 16, LB + 1], BF16)
            Ddiag = view(125, [128, 4, 128], BF16)
            BTj = view(128, [128, 8, 2, 4, 128], BF16)
            Ct = view(144, [128, 8, 2, 16, 32], BF16)
            Dt = view(176, [128, 8, 4, 128], BF16)
            wglu = view(184, [128, 4, 512], BF16)
            prm = view(188, [128, 40, 16], F32)
            Lpw = view(190.5, [128, 2, 16, 9], F32)
            braw = view(80, [128, 2, 16, 32], F32)
            craw = view(84, [128, 2, 16, 32], F32)
            tmp = [view(88 + 2 * i, [128, 16, 32], F32) for i in range(4)]
            Bj_bf = view(96, [128, 8, 2, 16, 32], BF16)
            C32b = view(120.5, [128, 2, 16, 32], BF16)
            ang = view(144, [128, NTB], F32)
            angk = view(148.25, [128, NTB], F32)
            angi = view(152.5, [128, NTB], I32)
            a9 = view(157, [128, 144], F32)
            a9k = view(157.75, [128, 144], F32)
            a9i = view(158.5, [128, 144], I32)
            t1s = view(96, [128, 4, 512], F32)
            sqb = view(104, [128, 4, 512], BF16)

            P = lambda i: prm[:, i, :]

            def sincos(a_, ak_, ai_, o_sin, o_cos, nm):
                add("dve", lambda e: e.tensor_scalar(out=ai_, in0=a_, scalar1=1.0 / TWO_PI, scalar2=None, op0=ALU.mult),
                    reads=[nm + "a"], writes=[nm + "i"])
                add("dve", lambda e: e.tensor_copy(out=ak_, in_=ai_), reads=[nm + "i"], writes=[nm + "k"])
                add("dve", lambda e: e.tensor_scalar(out=ak_, in0=ak_, scalar1=-TWO_PI, scalar2=None, op0=ALU.mult),
                    reads=[nm + "k"], writes=[nm + "k"])
                add("dve", lambda e: e.tensor_tensor(out=a_, in0=a_, in1=ak_, op=ALU.add), reads=[nm + "a", nm + "k"], writes=[nm + "a"])
                add("dve", lambda e: e.tensor_scalar(out=ak_, in0=a_, scalar1=math.pi, scalar2=-TWO_PI, op0=ALU.is_gt, op1=ALU.mult),
                    reads=[nm + "a"], writes=[nm + "k"])
                add("dve", lambda e: e.tensor_tensor(out=a_, in0=a_, in1=ak_, op=ALU.add), reads=[nm + "a", nm + "k"], writes=[nm + "a"])
                add("dve", lambda e: e.tensor_scalar(out=ak_, in0=a_, scalar1=-math.pi, scalar2=TWO_PI, op0=ALU.is_lt, op1=ALU.mult),
                    reads=[nm + "a"], writes=[nm + "k"])
                add("dve", lambda e: e.tensor_tensor(out=a_, in0=a_, in1=ak_, op=ALU.add), reads=[nm + "a", nm + "k"], writes=[nm + "a"])
                add("act", lambda e: e.activation(out=o_sin, in_=a_, func=AF.Sin), reads=[nm + "a"], writes=[nm + "sin"])
                add("dve", lambda e: e.tensor_scalar(out=ak_, in0=a_, scalar1=math.pi / 2, scalar2=-TWO_PI, op0=ALU.is_gt, op1=ALU.mult),
                    reads=[nm + "a"], writes=[nm + "k"])
                add("dve", lambda e: e.tensor_tensor(out=ak_, in0=ak_, in1=a_, op=ALU.add), reads=[nm + "a", nm + "k"], writes=[nm + "k"])
                add("act", lambda e: e.activation(out=o_cos, in_=ak_, func=AF.Sin, bias=halfpi[:]), reads=[nm + "k", "halfpi"], writes=[nm + "cos"])

            add("sp", lambda e, l=l: e.dma_start(out=P(0), in_=lamre_d[l]), writes=["p0"], dma=True)
            add("sp", lambda e, l=l: e.dma_start(out=P(1), in_=lamim_d[l]), writes=["p1"], dma=True)
            add("sp", lambda e, l=l: e.dma_start(out=P(2), in_=logdt_d[l]), writes=["p2"], dma=True)
            add("sp", lambda e, l=l: e.dma_start(out=braw[:, 0], in_=bre_d[l]), writes=["braw0"], dma=True)
            add("sp", lambda e, l=l: e.dma_start(out=braw[:, 1], in_=bim_d[l]), writes=["braw1"], dma=True)
            add("sp", lambda e, l=l: e.dma_start(out=craw[:, 0], in_=cre_d[l]), writes=["craw0"], dma=True)
            add("sp", lambda e, l=l: e.dma_start(out=craw[:, 1], in_=cim_d[l]), writes=["craw1"], dma=True)
            load_w_cast(wglu[:], w_glu_d[l].rearrange("(k p) n -> p k n", p=128), "wglu")
            add("pool", lambda e: e.memset(Dt[:], 0.0), writes=["Dt"])
            for c in range(4):
                add("pool", lambda e, c=c: e.tensor_scalar(out=Ddiag[:, c, :], in0=identf[:], scalar1=dsk[:, c:c + 1], scalar2=None, op0=ALU.mult),
                    reads=["identf", "dsk"], writes=["Ddiag"])
            add("act", lambda e: e.activation(out=P(3), in_=P(2), func=AF.Exp), reads=["p2"], writes=["p3"])
            add("dve", lambda e: e.tensor_tensor(out=P(4), in0=P(1), in1=P(3), op=ALU.mult), reads=["p1", "p3"], writes=["p4"])
            add("dve", lambda e: e.tensor_tensor(out=P(5), in0=P(0), in1=P(3), op=ALU.mult), reads=["p0", "p3"], writes=["p5"])
            add("dve", lambda e: e.tensor_scalar(out=P(7), in0=P(4), scalar1=8.0, scalar2=None, op0=ALU.mult), reads=["p4"], writes=["p7"])
            add("act", lambda e: e.activation(out=P(8), in_=P(5), func=AF.Exp, scale=8.0), reads=["p5"], writes=["p8"])
            a9v = a9.rearrange("p (j k) -> p j k", j=16)
            a9kv = a9k.rearrange("p (j k) -> p j k", j=16)
            io9 = iota_t[:, 0:9].unsqueeze(1).to_broadcast([128, 16, 9])
            add("dve", lambda e: e.tensor_tensor(out=a9kv, in0=P(5).unsqueeze(2).to_broadcast([128, 16, 9]), in1=io9, op=ALU.mult),
                reads=["p5", "iota"], writes=["rk0"])
            rk = tmp[2][:, :, 0:9]
            add("act", lambda e: e.activation(out=rk, in_=a9kv, func=AF.Exp), reads=["rk0"], writes=["rk"])
            add("dve", lambda e: e.tensor_tensor(out=a9v, in0=P(4).unsqueeze(2).to_broadcast([128, 16, 9]), in1=io9, op=ALU.mult),
                reads=["p4", "iota", "rk"], writes=["n9a"])
            s9 = view(88, [128, 144], F32)
            c9 = view(90, [128, 144], F32)
            sincos(a9, a9k, a9i, s9, c9, "n9")
            s9v = s9.rearrange("p (j k) -> p j k", j=16)
            c9v = c9.rearrange("p (j k) -> p j k", j=16)
            add("dve", lambda e: e.tensor_tensor(out=Lpw[:, 0], in0=c9v, in1=rk, op=ALU.mult), reads=["n9cos", "rk"], writes=["Lre"])
            add("dve", lambda e: e.tensor_tensor(out=Lpw[:, 1], in0=s9v, in1=rk, op=ALU.mult), reads=["n9sin", "rk"], writes=["Lim"])
            ang3 = ang.rearrange("p (j t) -> p j t", j=16)
            add("dve", lambda e: e.tensor_tensor(out=ang3, in0=P(7).unsqueeze(2).to_broadcast([128, 16, LB + 1]),
                                                  in1=iota_t[:, 0:LB + 1].unsqueeze(1).to_broadcast([128, 16, LB + 1]), op=ALU.mult),
                reads=["p7", "iota"], writes=["nTa"])
            sincos(ang, angk, angi, tsin.rearrange("p j t -> p (j t)"), tcos.rearrange("p j t -> p (j t)"), "nT")
            S.barrier()
            if stop == 31:
                raise _Stop()
            Lre = lambda k: Lpw[:, 0, :, k]
            Lim = lambda k: Lpw[:, 1, :, k]
            bc32 = lambda ap_: ap_.unsqueeze(2).to_broadcast([128, 16, 32])
            add("dve", lambda e: e.tensor_scalar(out=P(9), in0=Lre(1), scalar1=-1.0, scalar2=None, op0=ALU.add), reads=["Lre"], writes=["p9"])
            add("dve", lambda e: e.tensor_tensor(out=P(10), in0=P(0), in1=P(0), op=ALU.mult), reads=["p0"], writes=["p10"])
            add("dve", lambda e: e.tensor_tensor(out=P(11), in0=P(1), in1=P(1), op=ALU.mult), reads=["p1"], writes=["p11"])
            add("dve", lambda e: e.tensor_tensor(out=P(10), in0=P(10), in1=P(11), op=ALU.add), reads=["p10", "p11"], writes=["p10"])
            add("dve", lambda e: e.reciprocal(out=P(10), in_=P(10)), reads=["p10"], writes=["p10"])
            add("dve", lambda e: e.tensor_tensor(out=P(11), in0=P(9), in1=P(0), op=ALU.mult), reads=["p9", "p0"], writes=["p11"])
            add("dve", lambda e: e.tensor_tensor(out=P(12), in0=Lim(1), in1=P(1), op=ALU.mult), reads=["Lim", "p1"], writes=["p12"])
            add("dve", lambda e: e.tensor_tensor(out=P(11), in0=P(11), in1=P(12), op=ALU.add), reads=["p11", "p12"], writes=["p11"])
            add("dve", lambda e: e.tensor_tensor(out=P(11), in0=P(11), in1=P(10), op=ALU.mult), reads=["p11", "p10"], writes=["p11"])
            add("dve", lambda e: e.tensor_tensor(out=P(13), in0=Lim(1), in1=P(0), op=ALU.mult), reads=["Lim", "p0"], writes=["p13"])
            add("dve", lambda e: e.tensor_tensor(out=P(14), in0=P(9), in1=P(1), op=ALU.mult), reads=["p9", "p1"], writes=["p14"])
            add("dve", lambda e: e.tensor_tensor(out=P(13), in0=P(13), in1=P(14), op=ALU.subtract), reads=["p13", "p14"], writes=["p13"])
            add("dve", lambda e: e.tensor_tensor(out=P(13), in0=P(13), in1=P(10), op=ALU.mult), reads=["p13", "p10"], writes=["p13"])
            add("dve", lambda e: e.tensor_tensor(out=tmp[2], in0=braw[:, 0], in1=bc32(P(11)), op=ALU.mult), reads=["braw0", "p11", "rk", "n9sin", "n9cos"], writes=["tmp2"])
            add("dve", lambda e: e.tensor_tensor(out=tmp[3], in0=braw[:, 1], in1=bc32(P(13)), op=ALU.mult), reads=["braw1", "p13"], writes=["tmp3"])
            add("dve", lambda e: e.tensor_tensor(out=tmp[0], in0=tmp[2], in1=tmp[3], op=ALU.subtract), reads=["tmp2", "tmp3", "Lre", "Lim"], writes=["bb_re"])
            add("dve", lambda e: e.tensor_tensor(out=tmp[2], in0=braw[:, 1], in1=bc32(P(11)), op=ALU.mult), reads=["braw1", "p11", "bb_re"], writes=["tmp2"])
            add("dve", lambda e: e.tensor_tensor(out=tmp[3], in0=braw[:, 0], in1=bc32(P(13)), op=ALU.mult), reads=["braw0", "p13", "bb_re"], writes=["tmp3"])
            add("dve", lambda e: e.tensor_tensor(out=tmp[1], in0=tmp[2], in1=tmp[3], op=ALU.add), reads=["tmp2", "tmp3"], writes=["bb_im"])
            tA, tB = tmp[2], tmp[3]
            tC, tD = braw[:, 0], braw[:, 1]
            for j in range(8):
                k = 7 - j
                add("dve", lambda e, k=k: e.tensor_tensor(out=tA, in0=tmp[0], in1=bc32(Lre(k)), op=ALU.mult), reads=["bb_re", "Lre"], writes=["tmp2"])
                add("dve", lambda e, k=k: e.tensor_tensor(out=tB, in0=tmp[1], in1=bc32(Lim(k)), op=ALU.mult), reads=["bb_im", "Lim"], writes=["tmp3"])
                add("dve", lambda e, j=j: e.tensor_tensor(out=Bj_bf[:, j, 0], in0=tA, in1=tB, op=ALU.subtract), reads=["tmp2", "tmp3"], writes=[("Bj", j, 0)])
                add("dve", lambda e, k=k: e.tensor_tensor(out=tC, in0=tmp[1], in1=bc32(Lre(k)), op=ALU.mult), reads=["bb_im", "Lre", "bb_re"], writes=["braw0"])
                add("dve", lambda e, k=k: e.tensor_tensor(out=tD, in0=tmp[0], in1=bc32(Lim(k)), op=ALU.mult), reads=["bb_re", "Lim", "bb_im"], writes=["braw1"])
                add("dve", lambda e, j=j: e.tensor_tensor(out=Bj_bf[:, j, 1], in0=tC, in1=tD, op=ALU.add), reads=["braw0", "braw1"], writes=[("Bj", j, 1)])
            add("act", lambda e: e.activation(out=C32b[:, 0], in_=craw[:, 0], func=AF.Copy), reads=["craw0"], writes=["C32b0"])
            add("act", lambda e: e.activation(out=C32b[:, 1], in_=craw[:, 1], func=AF.Copy, scale=-1.0), reads=["craw1"], writes=["C32b1"])
            tE = view(120.5 + 2.0, [128, 16, 32], F32)
            tF = view(125.0 + 1.0, [128, 16, 32], F32)
            for t in range(8):
                k = t + 1
                add("dve", lambda e, k=k: e.tensor_tensor(out=tE, in0=craw[:, 0], in1=bc32(Lre(k)), op=ALU.mult), reads=["craw0", "Lre"], writes=["tE"])
                add("dve", lambda e, k=k: e.tensor_tensor(out=tF, in0=craw[:, 1], in1=bc32(Lim(k)), op=ALU.mult), reads=["craw1", "Lim"], writes=["tF"])
                add("dve", lambda e, t=t: e.tensor_tensor(out=Ct[:, t, 0], in0=tE, in1=tF, op=ALU.subtract), reads=["tE", "tF"], writes=[("Ct", t, 0)])
                add("dve", lambda e, k=k: e.tensor_tensor(out=tE, in0=craw[:, 0], in1=bc32(Lim(k)), op=ALU.mult), reads=["craw0", "Lim", ("Ct", t, 0)], writes=["tE"])
                add("dve", lambda e, k=k: e.tensor_tensor(out=tF, in0=craw[:, 1], in1=bc32(Lre(k)), op=ALU.mult), reads=["craw1", "Lre", ("Ct", t, 0)], writes=["tF"])
                add("dve", lambda e, t=t: e.scalar_tensor_tensor(out=Ct[:, t, 1], in0=tE, scalar=-1.0, in1=tF, op0=ALU.mult, op1=ALU.subtract),
                    reads=["tE", "tF"], writes=[("Ct", t, 1)])
            for j in range(8):
                for ri in range(2):
                    bank = (2 * j + ri) % 4
                    ptb = ps[bank][:].bitcast(BF16)
                    def f_tb(e, j=j, ri=ri, ptb=ptb):
                        last = None
                        for c in range(4):
                            last = e.transpose(out=ptb[:, c * 128:(c + 1) * 128],
                                               in_=Bj_bf[:, j, ri, 4 * c:4 * c + 4, :].rearrange("p a b -> p (a b)"), identity=ident[:])
                        return last
                    add("pe", f_tb, reads=[("Bj", j, ri), "ident"], writes=[PS(bank)])
                    add("act", lambda e, j=j, ri=ri, ptb=ptb: e.activation(out=BTj[:, j, ri].rearrange("p c m -> p (c m)"), in_=ptb[:, 0:512], func=AF.Copy),
                        reads=[PS(bank)], writes=[("BTj", j, ri)])
            for hb in range(2):
                def f_d(e, hb=hb):
                    last = None
                    for tl in range(4):
                        tau = 4 * hb + tl
                        jB = 7 - tau
                        for j in range(16):
                            c, q = j // 4, j % 4
                            col = tl * 128 + c * 32
                            for ri in range(2):
                                last = e.matmul(ps[4 + hb][32 * q:32 * q + 32, col:col + 32], lhsT=Bj_bf[:, jB, ri, j, :], rhs=C32b[:, ri, j, :],
                                                start=(ri == 0), stop=(ri == 1), tile_position=(0, 32 * q), skip_group_check=True)
                    return last
                add("pe", f_d, reads=[("Bj", j, ri) for j in range(8) for ri in range(2)] + ["C32b0", "C32b1"], writes=[PS(4 + hb)])
                for q in range(4):
                    add("dve", lambda e, hb=hb, q=q: e.tensor_copy(
                        out=Dt[32 * q:32 * q + 32, 4 * hb:4 * hb + 4, :, 32 * q:32 * q + 32],
                        in_=ps[4 + hb][32 * q:32 * q + 32, :].rearrange("p (t c m) -> p t c m", t=4, c=4)),
                        reads=[PS(4 + hb), "Dt"], writes=[("Dtw", hb, q)])
            add("dve", lambda e: e.tensor_tensor(out=Dt[:, 0, :, :], in0=Dt[:, 0, :, :], in1=Ddiag[:], op=ALU.add),
                reads=[("Dtw", 0, q) for q in range(4)] + ["Ddiag", "Dt"], writes=["Dt0"])
            add("dve", lambda e: e.memset(P(20), 0.0), writes=["car_re"])
            add("dve", lambda e: e.memset(P(21), 0.0), writes=["car_im"])
            S.barrier()
            if stop == 32:
                raise _Stop()
            add("pool", lambda e: e.memset(Sp_re[:, :, 0:1], 0.0), writes=["Sp_re"])
            add("pool", lambda e: e.memset(Sp_im[:, :, 0:1], 0.0), writes=["Sp_im"])
            cosT = tcos[:, :, 0:LB]
            sinT = tsin[:, :, 0:LB]
            cL = tcos[:, :, LB]
            sL = tsin[:, :, LB]
            def emit_fw(qi):
                tok0 = qi * 512
                def f_w(e, tok0=tok0):
                    last = None
                    for c in range(4):
                        for ri in range(2):
                            for jj in range(8):
                                for q in range(4):
                                    last = e.matmul(ps[q][:, (2 * c + ri) * LB:(2 * c + ri + 1) * LB],
                                                    lhsT=BTj[32 * q:32 * q + 32, jj, ri, c, :],
                                                    rhs=uT[32 * q:32 * q + 32, c, tok0 + jj:tok0 + jj + 8 * (LB - 1) + 1:8],
                                                    start=(jj == 0), stop=(jj == 7), tile_position=(32 * q, 0), skip_group_check=True)
                    return last
                add("pe", f_w, reads=[("uT", c_, qi) for c_ in range(4)], writes=[PS(q) for q in range(4)])
            emit_fw(0)
            for qi in range(NQ):
                tok0 = qi * 512
                for q in range(4):
                    pv = ps[q][:].rearrange("p (c r t) -> p c r t", c=4, r=2)
                    add("act", lambda e, pv=pv, q=q: e.activation(out=bufA[:, q:16:4, :], in_=pv[:, :, 0, :], func=AF.Copy),
                        reads=[PS(q)], writes=[("A", q)])
                    add("act", lambda e, pv=pv, q=q: e.activation(out=bufB[:, q:16:4, :], in_=pv[:, :, 1, :], func=AF.Copy),
                        reads=[PS(q)], writes=[("B", q)])
                if qi + 1 < NQ:
                    emit_fw(qi + 1)
                Aall = [("A", q) for q in range(4)]
                Ball = [("B", q) for q in range(4)]
                add("dve", lambda e: e.tensor_tensor(out=bufT1[:], in0=bufA[:], in1=cosT, op=ALU.mult), reads=Aall, writes=["T1"])
                add("dve", lambda e: e.tensor_tensor(out=bufT2[:], in0=bufB[:], in1=sinT, op=ALU.mult), reads=Ball, writes=["T2"])
                add("dve", lambda e: e.tensor_tensor(out=bufT1[:], in0=bufT1[:], in1=bufT2[:], op=ALU.add), reads=["T1", "T2"], writes=["T1"])
                add("dve", lambda e: e.tensor_tensor(out=bufT2[:], in0=bufB[:], in1=cosT, op=ALU.mult), reads=Ball + ["T1"], writes=["T2"])
                add("dve", lambda e: e.tensor_tensor(out=bufB[:], in0=bufA[:], in1=sinT, op=ALU.mult), reads=Aall + ["T2"], writes=Ball)
                add("dve", lambda e: e.tensor_tensor(out=bufT2[:], in0=bufT2[:], in1=bufB[:], op=ALU.subtract), reads=["T2"] + Ball, writes=["T2"])
                for j in range(16):
                    add("dve", lambda e, j=j: e.tensor_tensor_scan(out=bufA[:, j, :], data0=P(8)[:, j:j + 1].to_broadcast([128, LB]),
                                                                     data1=bufT1[:, j, :], initial=P(20)[:, j:j + 1], op0=ALU.mult, op1=ALU.add),
                        reads=["T1", "p8", "car_re"], writes=[("A", j % 4)])
                for j in range(16):
                    add("dve", lambda e, j=j: e.tensor_tensor_scan(out=bufB[:, j, :], data0=P(8)[:, j:j + 1].to_broadcast([128, LB]),
                                                                     data1=bufT2[:, j, :], initial=P(21)[:, j:j + 1], op0=ALU.mult, op1=ALU.add),
                        reads=["T2", "p8", "car_im"], writes=[("B", j % 4)])
                zl_re = bufA[:, :, LB - 1]
                zl_im = bufB[:, :, LB - 1]
                add("dve", lambda e: e.tensor_tensor(out=P(22), in0=zl_re, in1=cL, op=ALU.mult), reads=Aall, writes=["p22"])
                add("dve", lambda e: e.tensor_tensor(out=P(23), in0=zl_im, in1=sL, op=ALU.mult), reads=Ball, writes=["p23"])
                add("dve", lambda e: e.tensor_tensor(out=P(20), in0=P(22), in1=P(23), op=ALU.subtract), reads=["p22", "p23"], writes=["car_re"])
                add("dve", lambda e: e.tensor_tensor(out=P(22), in0=zl_re, in1=sL, op=ALU.mult), reads=Aall + ["car_re"], writes=["p22"])
                add("dve", lambda e: e.tensor_tensor(out=P(23), in0=zl_im, in1=cL, op=ALU.mult), reads=Ball + ["car_re"], writes=["p23"])
                add("dve", lambda e: e.tensor_tensor(out=P(21), in0=P(22), in1=P(23), op=ALU.add), reads=["p22", "p23"], writes=["car_im"])
                if qi > 0:
                    add("dve", lambda e: e.tensor_copy(out=Sp_re[:, :, 0], in_=Sp_re[:, :, LB]), reads=["Sp_re"], writes=["Sp_re"])
                    add("dve", lambda e: e.tensor_copy(out=Sp_im[:, :, 0], in_=Sp_im[:, :, LB]), reads=["Sp_im"], writes=["Sp_im"])
                add("dve", lambda e: e.tensor_tensor(out=bufT1[:], in0=bufA[:], in1=cosT, op=ALU.mult), reads=Aall + ["T1"], writes=["T1"])
                add("dve", lambda e: e.tensor_tensor(out=bufT2[:], in0=bufB[:], in1=sinT, op=ALU.mult), reads=Ball + ["T2"], writes=["T2"])
                add("dve", lambda e: e.tensor_tensor(out=Sp_re[:, :, 1:LB + 1], in0=bufT1[:], in1=bufT2[:], op=ALU.subtract), reads=["T1", "T2", "Sp_re"], writes=["Sp_re"])
                add("dve", lambda e: e.tensor_tensor(out=bufT1[:], in0=bufA[:], in1=sinT, op=ALU.mult), reads=Aall + ["Sp_re"], writes=["T1"])
                add("dve", lambda e: e.tensor_tensor(out=bufT2[:], in0=bufB[:], in1=cosT, op=ALU.mult), reads=Ball + ["Sp_re"], writes=["T2"])
                add("dve", lambda e: e.tensor_tensor(out=Sp_im[:, :, 1:LB + 1], in0=bufT1[:], in1=bufT2[:], op=ALU.add), reads=["T1", "T2", "Sp_im"], writes=["Sp_im"])
                for c in range(4):
                    def f_y(e, c=c, tok0=tok0):
                        last = None
                        first = True
                        for t in range(8):
                            reg = ps[4 + c][:, t * LB:(t + 1) * LB]
                            for jj in range(t + 1):
                                last = e.matmul(reg, lhsT=Dt[:, t - jj, c, :], rhs=uT[:, c, tok0 + jj:tok0 + jj + 8 * (LB - 1) + 1:8],
                                                start=first, stop=False, skip_group_check=True)
                                first = False
                        for t in range(8):
                            for ri, Sp_ in enumerate((Sp_re, Sp_im)):
                                for q in range(4):
                                    j = 4 * c + q
                                    reg = ps[4 + c][32 * q:32 * q + 32, t * LB:(t + 1) * LB]
                                    last = e.matmul(reg, lhsT=Ct[:, t, ri, j, :], rhs=Sp_[:, j, 0:LB], start=False,
                                                    stop=(t == 7 and ri == 1 and q == 3), tile_position=(0, 32 * q), skip_group_check=True)
                        return last
                    add("pe", f_y, reads=[("uT", c, qi), "Sp_re", "Sp_im", "Ddiag"], writes=[PS(4 + c)])
                    add("act", lambda e, c=c, tok0=tok0: e.activation(
                        out=mixS[:, c, tok0:tok0 + 512].rearrange("p (b t) -> p b t", t=8),
                        in_=ps[4 + c][:].rearrange("p (t b) -> p b t", t=8), func=AF.Gelu_apprx_tanh),
                        reads=[PS(4 + c)], writes=[("yg", c, qi)])
            S.barrier()
            if stop == 33:
                raise _Stop()
            load_w_cast(view(136, [128, 8, D], BF16)[:], w_out_d[l].rearrange("(k p) n -> p k n", p=128), "wout")
            t1s2 = [view(96, [128, 4, 512], F32), view(104, [128, 4, 512], F32)]
            sqb2 = [view(112, [128, 4, 512], BF16), view(116, [128, 4, 512], BF16)]
            for tb in range(4):
                tsl = slice(tb * 512, (tb + 1) * 512)
                t1s = t1s2[tb % 2]
                sqb = sqb2[tb % 2]
                sl_ = tb % 2
                def f_g(e, tsl=tsl):
                    last = None
                    for fc in range(4):
                        for k in range(4):
                            last = e.matmul(ps[fc][:], lhsT=wglu[:, k, fc * 128:(fc + 1) * 128], rhs=mixS[:, k, tsl], start=(k == 0), stop=(k == 3))
                    return last
                add("pe", f_g, reads=["wglu"] + [("yg", c, tb) for c in range(4)], writes=[PS(fc) for fc in range(4)])
                for fc in range(4):
                    add("act", lambda e, fc=fc, t1s=t1s: e.activation(out=t1s[:, fc, :], in_=ps[fc][:], func=AF.Sigmoid),
                        reads=[PS(fc)], writes=[("t1s", sl_, fc)])
                t1all = [("t1s", sl_, fc) for fc in range(4)]
                add("dve", lambda e, tsl=tsl, t1s=t1s: e.tensor_tensor(out=t1s[:], in0=t1s[:], in1=mixS[:, :, tsl], op=ALU.mult),
                    reads=t1all + [("yg", c, tb) for c in range(4)], writes=t1all)
                add("act", lambda e, t1s=t1s, sqb=sqb: e.activation(out=sqb[:], in_=t1s[:], func=AF.Square), reads=t1all, writes=[("sqb", sl_)])
                add("dve", lambda e, tsl=tsl, t1s=t1s: e.tensor_tensor(out=mixS[:, :, tsl], in0=t1s[:], in1=gns[:].unsqueeze(2).to_broadcast([128, 4, 512]), op=ALU.mult),
                    reads=t1all + ["gns"], writes=[("yg", c, tb) for c in range(4)] + [("mixS", 4 * tb + i) for i in range(4)])
                def f_ss(e, sqb=sqb, sl_=sl_):
                    last = None
                    for i in range(4):
                        for k in range(4):
                            last = e.matmul(ps[4 + sl_][:, i:i + 1], lhsT=sqb[:, k, i * 128:(i + 1) * 128], rhs=ones_b[:], start=(k == 0), stop=(k == 3),
                                            skip_group_check=True)
                    return last
                add("pe", f_ss, reads=[("sqb", sl_), "ones_b"], writes=[PS(4 + sl_)])
                add("act", lambda e, tb=tb, sl_=sl_: e.activation(out=rstd_s[:, 4 * tb:4 * tb + 4], in_=ps[4 + sl_][:, 0:4], func=AF.Copy),
                    reads=[PS(4 + sl_)], writes=[("rstd_s0", tb)])
            add("act", lambda e: e.activation(out=rstd_s[:], in_=rstd_s[:], func=AF.Sqrt, scale=1.0 / 512, bias=eps_t[:]),
                reads=[("rstd_s0", tb) for tb in range(4)] + ["eps"], writes=["rstd_s1"])
            add("dve", lambda e: e.reciprocal(out=rstd_s[:], in_=rstd_s[:]), reads=["rstd_s1"], writes=[("rstd_s", i) for i in range(NT)])
            S.barrier()
            if dbg and l == 0:
                dq = view(152, [128, 4, S_LEN], F32)
                add("dve", lambda e, dq=dq: e.tensor_copy(out=dq, in_=mixS), writes=["dq"])
                for cq in range(4):
                    add("sp", lambda e, cq=cq, dq=dq: e.dma_start(out=dbg_d["mixS"][:, cq, :], in_=dq[:, cq, :]), reads=["dq"], dma=True)
                add("sp", lambda e: e.dma_start(out=dbg_d["rs"], in_=rstd_s[:]), dma=True)
                S.barrier()

            if stop == 4:
                raise _Stop()
            wout = view(136, [128, 8, D], BF16)
            wgu = [view(112 + 12 * i, [128, 8, 512], BF16) for i in range(2)]
            wdn = [view(120 + 12 * i, [128, 2, D], BF16) for i in range(2)]
            w_gu_v = w_gu_d[l].rearrange("e (k p) n -> e p k n", p=128)
            w_dn_v = w_dn_d[l].rearrange("e (k p) n -> e p k n", p=128)
            for e_ in range(2):
                load_w_cast(wgu[e_][:], w_gu_v[e_], ("wgu", e_))
                load_w_cast(wdn[e_][:], w_dn_v[e_], ("wdn", e_))
            for t in range(NT):
                for hf in range(2):
                    bA, bS = 2 * hf, 2 * hf + 1
                    def f_o(e, t=t, hf=hf, bA=bA, bS=bS):
                        last = None
                        for k in range(4):
                            last = e.matmul(ps[bA][:], lhsT=mixA[:, k, t * 128:(t + 1) * 128], rhs=wout[:, k, hf * 512:(hf + 1) * 512],
                                            start=(k == 0), stop=(k == 3))
                        for k in range(4):
                            last = e.matmul(ps[bS][:], lhsT=mixS[:, k, t * 128:(t + 1) * 128], rhs=wout[:, 4 + k, hf * 512:(hf + 1) * 512],
                                            start=(k == 0), stop=(k == 3))
                        return last
                    add("pe", f_o, reads=["wout", ("mixA", t), ("mixS", t)], writes=[PS(bA), PS(bS)])
                    add("dve", lambda e, t=t, hf=hf, bA=bA: e.tensor_tensor(out=x[:, t, hf * 512:(hf + 1) * 512], in0=x[:, t, hf * 512:(hf + 1) * 512],
                                                                             in1=ps[bA][:], op=ALU.add), reads=[PS(bA), ("x", t)], writes=[("x", t)])
                    add("dve", lambda e, t=t, hf=hf, bS=bS: e.scalar_tensor_tensor(
                        out=x[:, t, hf * 512:(hf + 1) * 512], in0=ps[bS][:], scalar=rstd_s[:, t:t + 1], in1=x[:, t, hf * 512:(hf + 1) * 512],
                        op0=ALU.mult, op1=ALU.add), reads=[PS(bS), ("x", t), ("rstd_s", t)], writes=[("x", t)])
                    if hf == 1:
                        emit_sq(t)
            S.barrier()
            if dbg and l == 0:
                for t in range(NT):
                    add("sp", lambda e, t=t: e.dma_start(out=dbg_d["x1"][t * 128:(t + 1) * 128, :], in_=x[:, t, :]), reads=[("x", t)], dma=True)
                S.barrier()

            if stop == 5:
                raise _Stop()
            h2T = view(64, [128, 8, S_LEN], BF16)
            rmsnorm_T(h2T, ln2, "ln2", router=True, squares_done=True)
            S.barrier()
            if dbg and l == 0:
                add("sp", lambda e: e.dma_start(out=dbg_d["gates"], in_=gates[:]), dma=True)
                S.barrier()
            if stop == 6:
                raise _Stop()
            sgb = [view(168 + 0.5 * i, [128, 256], BF16) for i in range(2)]
            ab = [view(169 + 0.5 * i, [128, 256], BF16) for i in range(2)]
            aT = [view(170 + 0.5 * i, [128, 2, 128], BF16) for i in range(2)]
            steps = [(e_, t) for e_ in range(16) for t in range(NT)]
            nst = len(steps)

            def stage_G(i):
                e_, t = steps[i]
                sl = e_ % 2
                bank = i % 2
                def f(e):
                    last = None
                    for k in range(8):
                        last = e.matmul(ps[bank][:], lhsT=h2T[:, k, t * 128:(t + 1) * 128], rhs=wgu[sl][:, k, :], start=(k == 0), stop=(k == 7))
                    return last
                add("pe", f, reads=[("wgu", sl), ("hT", t)], writes=[PS(bank)])
                add("act", lambda e: e.activation(out=sgb[bank][:], in_=ps[bank][:, 0:256], func=AF.Silu), reads=[PS(bank)], writes=[("sgb", bank)])
                add("dve", lambda e: e.scalar_tensor_tensor(out=ab[bank][:], in0=ps[bank][:, 256:512], scalar=gates[:, t, e_:e_ + 1], in1=sgb[bank][:],
                                                            op0=ALU.mult, op1=ALU.mult), reads=[PS(bank), ("sgb", bank), ("gates", t)], writes=[("ab", bank)])

            def stage_T(i):
                bank = i % 2
                ptt = ps[2 + bank][:].bitcast(BF16)
                def f(e):
                    last = None
                    for fc in range(2):
                        last = e.transpose(out=ptt[:, fc * 128:(fc + 1) * 128], in_=ab[bank][:, fc * 128:(fc + 1) * 128], identity=ident[:])
                    return last
                add("pe", f, reads=[("ab", bank), "ident"], writes=[PS(2 + bank)])
                add("act", lambda e: e.activation(out=aT[bank][:].rearrange("p a b -> p (a b)"), in_=ptt[:, 0:256], func=AF.Copy),
                    reads=[PS(2 + bank)], writes=[("aT", bank)])

            def stage_D(i):
                e_, t = steps[i]
                sl = e_ % 2
                bank = i % 2
                b0 = 4 + 2 * bank
                def f(e):
                    last = None
                    for hf in range(2):
                        for fc in range(2):
                            last = e.matmul(ps[b0 + hf][:], lhsT=aT[bank][:, fc, :], rhs=wdn[sl][:, fc, hf * 512:(hf + 1) * 512],
                                            start=(fc == 0), stop=(fc == 1))
                    return last
                add("pe", f, reads=[("aT", bank), ("wdn", sl)], writes=[PS(b0), PS(b0 + 1)])
                for hf in range(2):
                    eng = "dve"
                    add(eng, lambda e, hf=hf: e.tensor_tensor(out=x[:, t, hf * 512:(hf + 1) * 512], in0=x[:, t, hf * 512:(hf + 1) * 512],
                                                              in1=ps[b0 + hf][:], op=ALU.add), reads=[PS(b0 + hf), ("x", t)], writes=[("x", t)])
                if e_ == 15 and l + 1 < L:
                    emit_sq(t)
                if t == NT - 1 and e_ + 2 < 16:
                    load_w_cast(wgu[sl][:], w_gu_v[e_ + 2], ("wgu", sl))
                    load_w_cast(wdn[sl][:], w_dn_v[e_ + 2], ("wdn", sl))

            for i in range(nst + 2):
                if i < nst:
                    stage_G(i)
                if 1 <= i <= nst:
                    stage_T(i - 1)
                if 2 <= i <= nst + 1:
                    stage_D(i - 2)
            S.barrier()

          except _Stop:
            S.barrier()

        gfin = view(64, [128, D], F32)
        add("sp", lambda e: e.dma_start(out=gfin, in_=fin_d.partition_broadcast(128)), writes=["gfin"], dma=True)
        ob = [view(72 + 4 * i, [128, D], F32) for i in range(2)]
        for t in range(NT):
            ss = sm[:, 0, 0:1]
            rs = sm[:, 0, 1:2]
            add("act", lambda e, t=t: e.activation(out=junk[:], in_=x[:, t, :], func=AF.Square, accum_out=ss),
                reads=[("x", t)], writes=["junk", "ss"])
            add("act", lambda e: e.activation(out=rs, in_=ss, func=AF.Sqrt, scale=1.0 / D, bias=eps_t[:]), reads=["ss", "eps"], writes=["rs0", "rs"])
            add("dve", lambda e: e.reciprocal(out=rs, in_=rs), reads=["rs0"], writes=["rs"])
            add("dve", lambda e, t=t: e.scalar_tensor_tensor(out=ob[t % 2], in0=x[:, t, :], scalar=rs, in1=gfin, op0=ALU.mult, op1=ALU.mult),
                reads=[("x", t), "rs", "gfin"], writes=[("ob", t % 2)])
            add("sp", lambda e, t=t: e.dma_start(out=out_d[t * 128:(t + 1) * 128, :], in_=ob[t % 2]), reads=[("ob", t % 2)], dma=True)

        S.emit(nc, es)
    return nc


def prep_inputs(inp, depth=DEPTH):
    f = lambda a: np.ascontiguousarray(np.asarray(a, dtype=np.float32))
    L = depth
    G, Pn, H = 32, 64, 16
    sh = {}
    sh["w_in"] = f(inp["w_in"][:L])
    sh["w_out"] = f(inp["w_out"][:L])
    sh["w_glu"] = f(inp["w_glu"][:L])
    sh["w_gu"] = f(np.concatenate([np.asarray(inp["w_gate"][:L]), np.asarray(inp["w_up"][:L])], axis=-1))
    sh["w_dn"] = f(inp["w_down"][:L])
    wre = np.transpose(np.asarray(inp["w_router_exp"][:L]), (0, 2, 1, 3)).reshape(L, D, 16)
    sh["w_r"] = f(np.concatenate([np.asarray(inp["w_router_grp"][:L]), wre], axis=-1))
    sh["b_r"] = f(np.concatenate([np.asarray(inp["b_router_grp"][:L]), np.asarray(inp["b_router_exp"][:L]).reshape(L, 16)], axis=-1)[:, None, :])
    pk = lambda a, k: f(np.transpose(np.asarray(a[:L]).reshape(L, k, 128), (0, 2, 1)))
    sh["ln1"] = pk(inp["ln1_g"], 8)
    sh["ln2"] = pk(inp["ln2_g"], 8)
    sh["gna"] = pk(inp["gn_attn"], 4)
    sh["gns"] = pk(inp["gn_ssm"], 4)
    sh["fin"] = f(np.asarray(inp["final_g"]).reshape(1, D))
    sh["dsk"] = pk(np.asarray(inp["d_skip"]).reshape(-1, 512), 4)
    sh["lamre"] = pk(np.asarray(inp["lam_re"]).reshape(-1, G * Pn), 16)
    sh["lamim"] = pk(np.asarray(inp["lam_im"]).reshape(-1, G * Pn), 16)
    ldt = np.repeat(np.asarray(inp["log_dt"])[:, :, None], Pn, axis=2).reshape(-1, G * Pn)
    sh["logdt"] = pk(ldt, 16)
    bre = np.zeros((L, 128, 16, 32), np.float32)
    bim = np.zeros((L, 128, 16, 32), np.float32)
    cre = np.zeros((L, 128, 16, 32), np.float32)
    cim = np.zeros((L, 128, 16, 32), np.float32)
    b_re = np.asarray(inp["b_re"]); b_im = np.asarray(inp["b_im"]); c_re = np.asarray(inp["c_re"]); c_im = np.asarray(inp["c_im"])
    for j in range(16):
        for gl in range(2):
            g = 2 * j + gl
            bre[:, gl * 64:(gl + 1) * 64, j, gl * 16:(gl + 1) * 16] = b_re[:L, g]
            bim[:, gl * 64:(gl + 1) * 64, j, gl * 16:(gl + 1) * 16] = b_im[:L, g]
            col = 16 * gl
            cre[:, gl * 64:(gl + 1) * 64, j, col:col + 16] = np.transpose(c_re[:L, g], (0, 2, 1))
            cim[:, gl * 64:(gl + 1) * 64, j, col:col + 16] = np.transpose(c_im[:L, g], (0, 2, 1))
    sh["bre"], sh["bim"], sh["cre"], sh["cim"] = bre, bim, cre, cim
    return sh


_NC_CACHE = {}


def kernel(**inputs):
    x = np.asarray(inputs["x"], dtype=np.float32)
    B = x.shape[0]
    sh = prep_inputs(inputs)
    if "nc" not in _NC_CACHE:
        _NC_CACHE["nc"] = build()
    nc = _NC_CACHE["nc"]
    in_maps = []
    for b in range(B):
        m = dict(sh)
        m["x"] = np.ascontiguousarray(x[b])
        in_maps.append(m)
    res = run_bass_kernel_spmd(nc, in_maps, core_ids=list(range(B)))
    out = np.stack([np.asarray(r["out"]) for r in res.results], axis=0)
    return out.astype(np.float32)
```

```python
import math
import numpy as np
import concourse.bass as bass
import concourse.mybir as mybir
from concourse.bass_utils import run_bass_kernel_spmd
from contextlib import ExitStack

F32 = mybir.dt.float32
BF16 = mybir.dt.bfloat16
I32 = mybir.dt.int32
ALU = mybir.AluOpType
AF = mybir.ActivationFunctionType
AX = mybir.AxisListType

NDMA_SLOTS = 6
DEPTH = 4
S_LEN = 2048
D = 1024
NT = 16
LC = 128
NCH = S_LEN // LC
TWO_PI = 2.0 * math.pi


class _Op:
    __slots__ = ("eng", "fn", "reads", "writes", "deps", "idx", "sig", "is_dma",
                 "slot", "val", "cnt")


class Sched:
    ENGS = ("pe", "act", "dve", "pool", "sp")

    def __init__(self, same_engine_sync=True):
        self.ops = []
        self.last_w = {}
        self.readers = {}
        self.same_engine_sync = same_engine_sync
        self.since_barrier = []

    def add(self, eng, fn, reads=(), writes=(), dma=False):
        op = _Op()
        op.eng = eng
        op.fn = fn
        op.reads = tuple(reads)
        op.writes = tuple(writes)
        op.is_dma = dma
        op.idx = len(self.ops)
        op.sig = False
        deps = set()
        for r in op.reads:
            w = self.last_w.get(r)
            if w is not None:
                deps.add(w)
        for w_ in op.writes:
            w = self.last_w.get(w_)
            if w is not None:
                deps.add(w)
            for rd in self.readers.get(w_, ()):
                deps.add(rd)
        deps.discard(op)
        op.deps = deps
        for r in op.reads:
            self.readers.setdefault(r, []).append(op)
        for w_ in op.writes:
            self.last_w[w_] = op
            self.readers[w_] = []
        self.ops.append(op)
        self.since_barrier.append(op)
        return op

    def barrier(self):
        last = {}
        dmas = []
        for op in self.since_barrier:
            if op.is_dma:
                dmas.append(op)
            elif op.fn is not None:
                last[op.eng] = op
        deps = set(last.values()) | set(dmas)
        self.since_barrier = []
        for e in self.ENGS:
            op = _Op()
            op.eng = e
            op.fn = None
            op.reads = ()
            op.writes = ()
            op.is_dma = False
            op.idx = len(self.ops)
            op.sig = False
            op.deps = set(d for d in deps)
            self.ops.append(op)

    def finalize(self):
        for op in self.ops:
            for d in op.deps:
                if d.is_dma:
                    d.sig = True
                elif d.eng == op.eng and not op.is_dma:
                    if op.eng == "pe":
                        continue
                    if self.same_engine_sync or op.fn is None:
                        d.sig = True
                else:
                    d.sig = True
        cnt = {e: 0 for e in self.ENGS}
        dcnt = {e: 0 for e in self.ENGS}
        for op in self.ops:
            if op.is_dma:
                j = dcnt[op.eng]
                dcnt[op.eng] += 1
                op.slot = j % NDMA_SLOTS
                op.val = 16 * (j // NDMA_SLOTS + 1)
                op.cnt = j
            elif op.sig:
                cnt[op.eng] += 1
                op.val = cnt[op.eng]

    def emit(self, nc, es):
        self.finalize()
        sems = {e: es.enter_context(nc.semaphore("s_" + e)) for e in self.ENGS}
        dsems = {}
        for e in ("sp", "act", "pool"):
            dsems[e] = [es.enter_context(nc.semaphore("d_%s%d" % (e, i))) for i in range(NDMA_SLOTS)]
        block = es.enter_context(nc.Block())
        per = {e: [op for op in self.ops if op.eng == e] for e in self.ENGS}

        def run(engname, eng):
            seen = {}

            def wait(key, sem, val):
                if seen.get(key, 0) >= val:
                    return
                seen[key] = val
                if getattr(self, "trace", None) is not None:
                    self.trace.append((engname, "wait", key, val))
                eng.wait_ge(sem, val)

            for op in per[engname]:
                need = {}
                for d in op.deps:
                    if d.is_dma:
                        key = ("d", d.eng, d.slot)
                        need[key] = max(need.get(key, 0), d.val)
                    else:
                        if d.eng == engname and not op.is_dma:
                            if engname == "pe" or not (self.same_engine_sync or op.fn is None):
                                continue
                        if not d.sig:
                            continue
                        key = ("e", d.eng)
                        need[key] = max(need.get(key, 0), d.val)
                if op.is_dma and op.cnt >= NDMA_SLOTS:
                    key = ("d", engname, op.slot)
                    need[key] = max(need.get(key, 0), op.val - 16)
                for key, val in need.items():
                    if key[0] == "d":
                        wait(key, dsems[key[1]][key[2]], val)
                    else:
                        wait(key, sems[key[1]], val)
                if getattr(self, "trace", None) is not None:
                    self.trace.append((engname, "op", op.idx, op.is_dma, getattr(op, "slot", None), getattr(op, "val", None), op.sig, op.writes))
                if op.fn is None:
                    continue
                inst = op.fn(eng)
                if op.is_dma:
                    inst.then_inc(dsems[engname][op.slot], 16)
                elif op.sig:
                    inst.then_inc(sems[engname], 1)
            lastd = {}
            for op in per[engname]:
                if op.is_dma:
                    lastd[op.slot] = op.val
            for slot, val in lastd.items():
                eng.wait_ge(dsems[engname][slot], val)

        @block.tensor
        def _(e):
            run("pe", e)

        @block.scalar
        def _(e):
            run("act", e)

        @block.vector
        def _(e):
            run("dve", e)

        @block.gpsimd
        def _(e):
            run("pool", e)

        @block.sync
        def _(e):
            run("sp", e)


class _Stop(Exception):
    pass


def build(depth=DEPTH, dbg=False, stop=99):
    nc = bass.Bass("TRN2", target_bir_lowering=False)
    L = depth

    def din(name, shape):
        return nc.dram_tensor(name, list(shape), F32, kind="ExternalInput").ap()

    x_d = din("x", [S_LEN, D])
    w_in_d = din("w_in", [L, D, 2048])
    w_out_d = din("w_out", [L, D, D])
    w_glu_d = din("w_glu", [L, 512, 512])
    w_gu_d = din("w_gu", [L, 16, D, 512])
    w_dn_d = din("w_dn", [L, 16, 256, D])
    w_r_d = din("w_r", [L, D, 20])
    b_r_d = din("b_r", [L, 1, 20])
    ln1_d = din("ln1", [L, 128, 8])
    ln2_d = din("ln2", [L, 128, 8])
    gna_d = din("gna", [L, 128, 4])
    gns_d = din("gns", [L, 128, 4])
    fin_d = din("fin", [1, D])
    lamre_d = din("lamre", [L, 128, 16])
    lamim_d = din("lamim", [L, 128, 16])
    logdt_d = din("logdt", [L, 128, 16])
    bre_d = din("bre", [L, 128, 16, 32])
    bim_d = din("bim", [L, 128, 16, 32])
    cre_d = din("cre", [L, 128, 16, 32])
    cim_d = din("cim", [L, 128, 16, 32])
    dsk_d = din("dsk", [L, 128, 4])
    out_d = nc.dram_tensor("out", [S_LEN, D], F32, kind="ExternalOutput").ap()
    scr_d = nc.dram_tensor("scr", [2, S_LEN, 520], F32, kind="Internal").ap()
    dbg_d = {}
    if dbg:
        dbg_d["x1"] = nc.dram_tensor("dbg_x1", [S_LEN, D], F32, kind="ExternalOutput").ap()
        dbg_d["mixA"] = nc.dram_tensor("dbg_mixA", [128, 4, S_LEN], F32, kind="ExternalOutput").ap()
        dbg_d["mixS"] = nc.dram_tensor("dbg_mixS", [128, 4, S_LEN], F32, kind="ExternalOutput").ap()
        dbg_d["gates"] = nc.dram_tensor("dbg_gates", [128, NT, 16], F32, kind="ExternalOutput").ap()
        dbg_d["qT"] = nc.dram_tensor("dbg_qT", [128, 4, S_LEN], F32, kind="ExternalOutput").ap()
        dbg_d["uT"] = nc.dram_tensor("dbg_uT", [128, 4, S_LEN], F32, kind="ExternalOutput").ap()
        dbg_d["rs"] = nc.dram_tensor("dbg_rs", [128, NT], F32, kind="ExternalOutput").ap()

    S = Sched()
    if dbg:
        S.trace = []
        build.last_sched = S
    add = S.add
    with ExitStack() as es:
        def sb(name, shape, dt):
            return es.enter_context(nc.sbuf_tensor("sb_" + name, list(shape), dt))

        ARENA_W = 48 * 1024
        arena = sb("arena", [128, ARENA_W], F32)

        def view(off_kib, shape, dt):
            nwords_per = 1 if dt in (F32, I32) else 0.5
            n = int(np.prod(shape[1:]) * nwords_per)
            off = int(off_kib * 256)
            assert off + n <= ARENA_W, (off_kib, shape)
            a = arena[:, off:off + n]
            if dt != F32:
                a = a.bitcast(dt)
            if len(shape) > 2:
                names = ["d%d" % i for i in range(len(shape) - 1)]
                pat = "p (" + " ".join(names) + ") -> p " + " ".join(names)
                kw = {names[i]: int(shape[1 + i]) for i in range(len(names) - 1)}
                a = a.rearrange(pat, **kw)
            return a

        x = view(0, [128, NT, D], F32)
        ident = sb("ident", [128, 128], BF16)
        identf = sb("identf", [128, 128], F32)
        mask = sb("mask", [128, 256], BF16)
        ones_b = sb("ones_b", [128, 1], BF16)
        iota_t = sb("iota_t", [128, LC + 1], F32)
        eps_t = sb("eps_t", [128, 1], F32)
        halfpi = sb("halfpi", [128, 1], F32)
        ln1 = sb("ln1", [128, 8], F32)
        ln2 = sb("ln2", [128, 8], F32)
        gna = sb("gna", [128, 4], F32)
        gns = sb("gns", [128, 4], F32)
        dsk = sb("dsk", [128, 4], F32)
        b_r = sb("b_r", [128, 20], F32)
        w_r = sb("w_r", [128, 8, 20], F32)
        gates = sb("gates", [128, NT, 16], F32)
        rstd_s = sb("rstd_s", [128, NT], F32)
        sm = sb("sm", [128, 16, 32], F32)
        junk = sb("junk", [128, D], BF16)
        nrm = sb("nrm", [128, 32], F32)
        lall = sb("lall", [128, NT, 20], F32)
        rt = sb("rt", [128, 960], F32)
        maskf = rt[:, 0:256]
        onb_p = sb("onb_p", [128, 512], BF16)
        onb_p2 = sb("onb_p2", [128, 512], BF16)
        ps = [es.enter_context(nc.psum_tensor("ps%d" % i, [128, 512], F32)) for i in range(8)]

        def PS(i):
            return ("ps", i)

        add("pool", lambda e: e.memset(identf[:], 0.0), writes=["identf"])
        add("pool", lambda e: e.affine_select(out=identf[:], in_=identf[:], pattern=[[-1, 128]],
                                              compare_op=ALU.not_equal, fill=1.0, base=0, channel_multiplier=1),
            reads=["identf"], writes=["identf"])
        add("pool", lambda e: e.tensor_copy(out=ident[:], in_=identf[:]), reads=["identf"], writes=["ident"])
        add("pool", lambda e: e.memset(maskf, 1.0), writes=["maskf"])
        add("pool", lambda e: e.affine_select(out=maskf[:, 0:128], in_=maskf[:, 0:128], pattern=[[1, 128]],
                                              compare_op=ALU.is_ge, fill=0.0, base=0, channel_multiplier=-1),
            reads=["maskf"], writes=["maskf"])
        add("pool", lambda e: e.affine_select(out=maskf[:, 128:256], in_=maskf[:, 128:256], pattern=[[-1, 128]],
                                              compare_op=ALU.is_ge, fill=0.0, base=0, channel_multiplier=1),
            reads=["maskf"], writes=["maskf"])
        add("pool", lambda e: e.tensor_scalar(out=mask[:], in0=maskf, scalar1=-1.0, scalar2=30000.0, op0=ALU.add, op1=ALU.mult), reads=["maskf"], writes=["mask"])
        add("pool", lambda e: e.memset(ones_b[:], 1.0), writes=["ones_b"])
        add("pool", lambda e: e.memset(eps_t[:], 1e-6), writes=["eps"])
        add("pool", lambda e: e.memset(halfpi[:], math.pi / 2), writes=["halfpi"])
        add("pool", lambda e: e.iota(iota_t[:], pattern=[[1, LC + 1]], base=0, channel_multiplier=0,
                                     allow_small_or_imprecise_dtypes=True), writes=["iota"])
        for t in range(NT):
            add("sp", lambda e, t=t: e.dma_start(out=x[:, t, :], in_=x_d[t * 128:(t + 1) * 128, :]),
                writes=[("x", t)], dma=True)

        def emit_sq(t):
            add("act", lambda e, t=t: e.activation(out=junk[:], in_=x[:, t, :], func=AF.Square, accum_out=nrm[:, t:t + 1]),
                reads=[("x", t)], writes=["junk", ("ss", t)])

        def rmsnorm_T(hT, g_sb, gname, router, squares_done=False):
            xs2 = [view(160, [128, D], F32), view(164, [128, D], F32)]
            h32 = view(168, [128, 8, 128], F32)
            ssall = nrm[:, 0:16]
            rsall = nrm[:, 16:32]
            if not squares_done:
                for t in range(NT):
                    emit_sq(t)
            add("act", lambda e: e.activation(out=rsall, in_=ssall, func=AF.Sqrt, scale=1.0 / D, bias=eps_t[:]),
                reads=[("ss", t) for t in range(NT)] + ["eps"], writes=["rs0", "rs"])
            add("dve", lambda e: e.reciprocal(out=rsall, in_=rsall), reads=["rs0"], writes=["rs"])
            xsb2 = [view(160, [128, D], BF16), view(164, [128, D], BF16)]

            def st_S(t):
                xs = xs2[t % 2] if router else xsb2[t % 2]
                add("dve", lambda e, t=t, xs=xs: e.tensor_scalar(out=xs, in0=x[:, t, :], scalar1=rsall[:, t:t + 1], scalar2=None, op0=ALU.mult),
                    reads=[("x", t), "rs"], writes=[("xs", t % 2)])

            def st_X(t):
                xs = xs2[t % 2]
                b0 = 2 * (t % 2)
                if not router:
                    xsb = xsb2[t % 2]
                    pbf = ps[b0][:].bitcast(BF16)
                    def f_trb(e, xsb=xsb, pbf=pbf):
                        last = None
                        for k in range(8):
                            last = e.transpose(out=pbf[:, k * 128:(k + 1) * 128], in_=xsb[:, k * 128:(k + 1) * 128], identity=ident[:])
                        return last
                    add("pe", f_trb, reads=[("xs", t % 2), "ident"], writes=[PS(b0)])
                    return
                def f_tr(e, xs=xs, b0=b0):
                    last = None
                    for k in range(8):
                        last = e.transpose(out=ps[b0 + k // 4][:, (k % 4) * 128:(k % 4 + 1) * 128], in_=xs[:, k * 128:(k + 1) * 128],
                                           identity=identf[:])
                    return last
                add("pe", f_tr, reads=[("xs", t % 2), "identf"], writes=[PS(b0), PS(b0 + 1)])

            def st_E(t):
                b0 = 2 * (t % 2)
                for hlf in range(2):
                    if router:
                        add("dve", lambda e, hlf=hlf, b0=b0: e.tensor_tensor(
                            out=h32[:, 4 * hlf:4 * hlf + 4, :], in0=ps[b0 + hlf][:].rearrange("p (a b) -> p a b", a=4),
                            in1=g_sb[:, 4 * hlf:4 * hlf + 4].unsqueeze(2).to_broadcast([128, 4, 128]), op=ALU.mult),
                            reads=[PS(b0 + hlf), gname], writes=[("h32", hlf)])
                        add("act", lambda e, hlf=hlf, t=t: e.activation(out=hT[:, 4 * hlf:4 * hlf + 4, t * 128:(t + 1) * 128],
                                                                       in_=h32[:, 4 * hlf:4 * hlf + 4, :], func=AF.Copy),
                            reads=[("h32", hlf)], writes=[("hT", t)])
                    elif hlf == 0:
                        add("dve", lambda e, t=t, b0=b0: e.tensor_tensor(
                            out=hT[:, :, t * 128:(t + 1) * 128], in0=ps[b0][:].bitcast(BF16).rearrange("p (a b) -> p a b", a=8),
                            in1=g_sb[:].unsqueeze(2).to_broadcast([128, 8, 128]), op=ALU.mult),
                            reads=[PS(b0), gname], writes=[("hT", t)])
                if router:
                    def f_mm(e, t=t):
                        last = None
                        for k in range(8):
                            last = e.matmul(ps[4 + t % 2][:, 0:20], lhsT=h32[:, k, :], rhs=w_r[:, k, :], start=(k == 0), stop=(k == 7))
                        return last
                    add("pe", f_mm, reads=[("h32", 0), ("h32", 1), "w_r"], writes=[PS(4 + t % 2)])
                    add("dve", lambda e, t=t: e.tensor_tensor(out=lall[:, t, :], in0=ps[4 + t % 2][:, 0:20], in1=b_r[:], op=ALU.add),
                        reads=[PS(4 + t % 2), "b_r"], writes=[("lall", t)])

            st_S(0)
            st_X(0)
            for t in range(NT):
                if t + 1 < NT:
                    st_S(t + 1)
                    st_X(t + 1)
                st_E(t)
            if router:
                router_batched()

        def router_batched():
            T_ = NT
            lall_r = [("lall", t) for t in range(NT)]
            lg = lall[:, :, 0:4]
            le = lall[:, :, 4:20].rearrange("p t (g e) -> p t g e", g=4)
            _off = [0]
            def V(k):
                o = _off[0]
                _off[0] += 16 * k
                return rt[:, o:o + 16 * k].rearrange("p (t k) -> p t k", t=16)
            m = V(1); ohg = V(4); eg = V(4); sg = V(1); g1 = V(1)
            sel = V(4); v1 = V(1); oh1 = V(4); sel2 = V(4); v2 = V(1); oh2 = V(4)
            dv = V(1); ex = V(1); w1 = V(1); w2 = V(1); inner = V(4); inner2 = V(4)
            t16 = V(16).rearrange("p t (g e) -> p t g e", g=4)
            b4 = lambda ap_: ap_.to_broadcast([128, 16, 4])
            add("dve", lambda e: e.tensor_reduce(out=m, in_=lg, axis=AX.X, op=ALU.max), reads=lall_r, writes=["r_m"])
            add("dve", lambda e: e.tensor_tensor(out=ohg, in0=lg, in1=b4(m), op=ALU.is_equal), reads=lall_r + ["r_m"], writes=["r_ohg"])
            add("dve", lambda e: e.tensor_tensor(out=eg, in0=lg, in1=b4(m), op=ALU.subtract), reads=lall_r + ["r_m"], writes=["r_eg0"])
            add("act", lambda e: e.activation(out=eg, in_=eg, func=AF.Exp), reads=["r_eg0"], writes=["r_eg"])
            add("dve", lambda e: e.tensor_reduce(out=sg, in_=eg, axis=AX.X, op=ALU.add), reads=["r_eg"], writes=["r_sg"])
            add("dve", lambda e: e.reciprocal(out=g1, in_=sg), reads=["r_sg"], writes=["r_g1"])
            add("dve", lambda e: e.tensor_tensor(out=t16, in0=le, in1=ohg.unsqueeze(3).to_broadcast([128, 16, 4, 4]), op=ALU.mult),
                reads=lall_r + ["r_ohg"], writes=["r_t16"])
            add("dve", lambda e: e.tensor_reduce(out=sel, in_=t16.rearrange("p t g e -> p t e g"), axis=AX.X, op=ALU.add),
                reads=["r_t16"], writes=["r_sel"])
            add("dve", lambda e: e.tensor_reduce(out=v1, in_=sel, axis=AX.X, op=ALU.max), reads=["r_sel"], writes=["r_v1"])
            add("dve", lambda e: e.tensor_tensor(out=oh1, in0=sel, in1=b4(v1), op=ALU.is_equal), reads=["r_sel", "r_v1"], writes=["r_oh1"])
            add("dve", lambda e: e.scalar_tensor_tensor(out=sel2, in0=oh1, scalar=-1e30, in1=sel, op0=ALU.mult, op1=ALU.add),
                reads=["r_oh1", "r_sel"], writes=["r_sel2"])
            add("dve", lambda e: e.tensor_reduce(out=v2, in_=sel2, axis=AX.X, op=ALU.max), reads=["r_sel2"], writes=["r_v2"])
            add("dve", lambda e: e.tensor_tensor(out=oh2, in0=sel2, in1=b4(v2), op=ALU.is_equal), reads=["r_sel2", "r_v2"], writes=["r_oh2"])
            add("dve", lambda e: e.tensor_tensor(out=dv, in0=v2, in1=v1, op=ALU.subtract), reads=["r_v1", "r_v2"], writes=["r_dv"])
            add("act", lambda e: e.activation(out=ex, in_=dv, func=AF.Exp), reads=["r_dv"], writes=["r_ex"])
            add("dve", lambda e: e.tensor_scalar(out=ex, in0=ex, scalar1=1.0, scalar2=None, op0=ALU.add), reads=["r_ex"], writes=["r_den"])
            add("dve", lambda e: e.reciprocal(out=ex, in_=ex), reads=["r_den"], writes=["r_rden"])
            add("dve", lambda e: e.tensor_tensor(out=w1, in0=ex, in1=g1, op=ALU.mult), reads=["r_rden", "r_g1"], writes=["r_w1"])
            add("dve", lambda e: e.tensor_tensor(out=w2, in0=g1, in1=w1, op=ALU.subtract), reads=["r_w1", "r_g1"], writes=["r_w2"])
            add("dve", lambda e: e.tensor_tensor(out=inner, in0=oh1, in1=b4(w1), op=ALU.mult), reads=["r_oh1", "r_w1"], writes=["r_in0"])
            add("dve", lambda e: e.tensor_tensor(out=inner2, in0=oh2, in1=b4(w2), op=ALU.mult), reads=["r_oh2", "r_w2"], writes=["r_in1"])
            add("dve", lambda e: e.tensor_tensor(out=inner, in0=inner, in1=inner2, op=ALU.add), reads=["r_in0", "r_in1"], writes=["r_in"])
            add("dve", lambda e: e.tensor_tensor(out=gates[:].rearrange("p t (g e) -> p t g e", g=4),
                                                 in0=ohg.unsqueeze(3).to_broadcast([128, 16, 4, 4]),
                                                 in1=inner.unsqueeze(2).to_broadcast([128, 16, 4, 4]), op=ALU.mult),
                reads=["r_ohg", "r_in"], writes=[("gates", t) for t in range(NT)])

        def load_w_cast(dst, src, tok):
            add("pool", lambda e: e.dma_start(out=dst, in_=src), writes=[tok], dma=True)

        for l in range(L):
          try:
            for (dst, src, nm) in ((ln1, ln1_d, "ln1"), (ln2, ln2_d, "ln2"), (gna, gna_d, "gna"), (gns, gns_d, "gns"),
                                   (dsk, dsk_d, "dsk")):
                add("sp", lambda e, dst=dst, src=src, l=l: e.dma_start(out=dst[:], in_=src[l]), writes=[nm], dma=True)
            add("sp", lambda e, l=l: e.dma_start(out=b_r[:], in_=b_r_d[l].partition_broadcast(128)), writes=["b_r"], dma=True)
            add("sp", lambda e, l=l: e.dma_start(out=w_r[:], in_=w_r_d[l].rearrange("(k p) n -> p k n", p=128)), writes=["w_r"], dma=True)

            hT = view(64, [128, 8, S_LEN], BF16)
            wsl = [view(96, [128, 8, 512], BF16), view(104, [128, 8, 512], BF16)]
            qT = view(112, [128, 4, S_LEN], BF16)
            kT = view(128, [128, 4, S_LEN], BF16)
            vT = view(144, [128, 4, S_LEN], BF16)
            uT = view(160, [128, 4, S_LEN], BF16)
            w_in_v = w_in_d[l].rearrange("(k p) n -> p k n", p=128)
            for c in range(2):
                load_w_cast(wsl[c][:], w_in_v[:, :, c * 512:(c + 1) * 512], ("wsl", c))
            rmsnorm_T(hT, ln1, "ln1", router=False, squares_done=(l > 0))
            S.barrier()
            if stop == 1:
                raise _Stop()
            dsts = [qT, kT, vT, uT]
            names = ["qT", "kT", "vT", "uT"]
            for c in range(4):
                for fc in range(4):
                    for tb in range(4):
                        bank = (fc * 4 + tb) % 4
                        def f_mm(e, c=c, fc=fc, tb=tb, bank=bank):
                            last = None
                            for k in range(8):
                                last = e.matmul(ps[bank][:], lhsT=wsl[c % 2][:, k, fc * 128:(fc + 1) * 128],
                                                rhs=hT[:, k, tb * 512:(tb + 1) * 512], start=(k == 0), stop=(k == 7))
                            return last
                        add("pe", f_mm, reads=[("wsl", c % 2)] + [("hT", t) for t in range(4 * tb, 4 * tb + 4)], writes=[PS(bank)])
                        eng = "act" if (tb % 2 == 0) else "dve"
                        if eng == "act":
                            add("act", lambda e, c=c, fc=fc, tb=tb, bank=bank: e.activation(
                                out=dsts[c][:, fc, tb * 512:(tb + 1) * 512], in_=ps[bank][:], func=AF.Copy),
                                reads=[PS(bank)], writes=[(names[c], fc, tb)])
                        else:
                            add("dve", lambda e, c=c, fc=fc, tb=tb, bank=bank: e.tensor_copy(
                                out=dsts[c][:, fc, tb * 512:(tb + 1) * 512], in_=ps[bank][:]),
                                reads=[PS(bank)], writes=[(names[c], fc, tb)])
                if c + 2 < 4:
                    load_w_cast(wsl[c % 2][:], w_in_v[:, :, (c + 2) * 512:(c + 3) * 512], ("wsl", c % 2))
            S.barrier()
            if dbg and l == 0:
                dq = view(64, [128, 4, S_LEN], F32)
                for (srcT, nm) in ((qT, "qT"), (uT, "uT")):
                    add("dve", lambda e, srcT=srcT, dq=dq: e.tensor_copy(out=dq, in_=srcT), writes=["dq"])
                    for cq in range(4):
                        add("sp", lambda e, nm=nm, cq=cq, dq=dq: e.dma_start(out=dbg_d[nm][:, cq, :], in_=dq[:, cq, :]), reads=["dq"], dma=True)
                    S.barrier()

            if stop == 2:
                raise _Stop()
            mixA = view(64, [128, 4, S_LEN], BF16)
            Vb = [view(80 + 1.25 * i, [128, 8, 65], BF16) for i in range(4)]
            pT = [view(85 + 4 * i, [128, 4, 2, 256], BF16) for i in range(4)]
            ost = [view(101 + 2.25 * i, [128, 8, 65], F32) for i in range(2)]
            cmb = [view(105.5 + 2.25 * i, [128, 8, 65], F32) for i in range(2)]
            onrm = view(110, [128, 8, 64], F32)
            onb = onb_p
            for i in range(4):
                add("pool", lambda e, i=i: e.memset(Vb[i][:, :, 64:65], 1.0), writes=[("Vb", i)])
            blk_ctr = 0
            pending = None
            tails = []
            for bi, dil in enumerate((4, 16, 1)):
                nbc = 16 // dil
                for b in range(16):
                    r, n = b // nbc, b % nbc
                    t0 = 128 * n * dil + r
                    has_next = (n < nbc - 1)
                    has_prev = (n > 0)
                    nq = 256 if has_next else 128
                    vslot = blk_ctr % 4
                    ptr = ps[4][:].bitcast(BF16)
                    def f_vt(e, t0=t0, dil=dil):
                        last = None
                        for hp in range(4):
                            last = e.transpose(out=ptr[:, hp * 128:(hp + 1) * 128],
                                               in_=vT[:, hp, t0:t0 + 127 * dil + 1:dil], identity=ident[:])
                        return last
                    add("pe", f_vt, reads=[("vT", hp, tb) for hp in range(4) for tb in range(4)] + ["ident"], writes=[PS(4)])
                    add("act", lambda e, vslot=vslot: e.activation(
                        out=Vb[vslot][:, :, 0:64], in_=ptr[:, 0:512].rearrange("p (h c) -> p h c", h=8), func=AF.Copy),
                        reads=[PS(4)], writes=[("Vb", vslot)])
                    pslot = blk_ctr % 4
                    for hpp in range(2):
                        def f_sc(e, hpp=hpp, t0=t0, dil=dil, nq=nq):
                            last = None
                            for hpo in range(2):
                                hp = 2 * hpp + hpo
                                for hh in range(2):
                                    last = e.matmul(ps[2 * hpp + hh][:, hpo * 256:hpo * 256 + nq],
                                                    lhsT=kT[hh * 64:(hh + 1) * 64, hp, t0:t0 + 127 * dil + 1:dil],
                                                    rhs=qT[hh * 64:(hh + 1) * 64, hp, t0:t0 + (nq - 1) * dil + 1:dil], start=(hpo == 0), stop=False,
                                                    skip_group_check=True)
                            for hpo in range(2):
                                for hh in range(2):
                                    last = e.matmul(ps[2 * hpp + hh][:, hpo * 256:hpo * 256 + nq], lhsT=ident[:], rhs=mask[:, 0:nq],
                                                    start=False, stop=True, skip_group_check=True)
                            return last
                        add("pe", f_sc, reads=[("kT", hp, tb) for hp in (2 * hpp, 2 * hpp + 1) for tb in range(4)] +
                            [("qT", hp, tb) for hp in (2 * hpp, 2 * hpp + 1) for tb in range(4)] + ["mask", "ident"], writes=[PS(2 * hpp), PS(2 * hpp + 1)])
                        for hh in range(2):
                            bank = 2 * hpp + hh
                            add("act", lambda e, pslot=pslot, bank=bank, nq=nq, hpp=hpp, hh=hh: e.activation(
                                out=pT[pslot][:, 2 * hpp:2 * hpp + 2, hh, 0:nq], in_=ps[bank][:].rearrange("p (h q) -> p h q", h=2)[:, :, 0:nq],
                                func=AF.Exp, scale=0.125), reads=[PS(bank)], writes=[("pT", pslot, hpp, hh)])
                    pall = [("pT", pslot, hpp, hh) for hpp in range(2) for hh in range(2)]
                    pass
                    def back(blk_ctr=blk_ctr, has_prev=has_prev, vslot=vslot, dil=dil, bi=bi, b=b, t0=t0):
                        def f_pv(e, blk_ctr=blk_ctr, has_prev=has_prev, vslot=vslot):
                            last = None
                            for h in range(8):
                                hp, hh = h // 2, h % 2
                                bank = 5 + h // 4
                                col = (h % 4) * 65
                                first = True
                                if has_prev:
                                    pprev = (blk_ctr - 1) % 4
                                    vprev = (blk_ctr - 1) % 4
                                    last = e.matmul(ps[bank][:, col:col + 65], lhsT=pT[pprev][:, hp, hh, 128:256],
                                                    rhs=Vb[vprev][:, h, :], start=True, stop=False)
                                    first = False
                                pcur = blk_ctr % 4
                                last = e.matmul(ps[bank][:, col:col + 65], lhsT=pT[pcur][:, hp, hh, 0:128],
                                                rhs=Vb[vslot][:, h, :], start=first, stop=True)
                            return last
                        rd = [("pT", blk_ctr % 4, a_, b_) for a_ in range(2) for b_ in range(2)] + [("Vb", vslot)]
                        if has_prev:
                            rd += [("pT", (blk_ctr - 1) % 4, a_, b_) for a_ in range(2) for b_ in range(2)] + [("Vb", (blk_ctr - 1) % 4)]
                        add("pe", f_pv, reads=rd, writes=[PS(5), PS(6)])
                        oslot = blk_ctr % 2
                        add("act", lambda e, oslot=oslot: e.activation(
                            out=ost[oslot][:, 0:4, :], in_=ps[5][:, 0:260].rearrange("p (h c) -> p h c", h=4), func=AF.Copy),
                            reads=[PS(5)], writes=[("ost", oslot, 0)])
                        add("dve", lambda e, oslot=oslot: e.tensor_copy(
                            out=ost[oslot][:, 4:8, :], in_=ps[6][:, 0:260].rearrange("p (h c) -> p h c", h=4)),
                            reads=[PS(6)], writes=[("ost", oslot, 1)])
                        if dil != 1:
                            dst = scr_d[bi, t0:t0 + 127 * dil + 1:dil, :]
                            add("sp", lambda e, dst=dst, oslot=oslot: e.dma_start(out=dst, in_=ost[oslot][:].rearrange("p h c -> p (h c)")),
                                reads=[("ost", oslot, 0), ("ost", oslot, 1)], writes=[("scr", bi, b)], dma=True)
                        else:
                            for j in range(2):
                                add("sp", lambda e, j=j, b=b: e.dma_start(out=cmb[j][:].rearrange("p h c -> p (h c)"),
                                                                          in_=scr_d[j, b * 128:(b + 1) * 128, :]),
                                    reads=[("scr", j, bb) for bb in range(16)], writes=[("cmb", j)], dma=True)
                            add("dve", lambda e: e.tensor_tensor(out=cmb[0][:], in0=cmb[0][:], in1=cmb[1][:], op=ALU.add),
                                reads=[("cmb", 0), ("cmb", 1)], writes=[("cmb", 0)])
                            add("dve", lambda e, oslot=oslot: e.tensor_tensor(out=cmb[0][:], in0=cmb[0][:], in1=ost[oslot][:], op=ALU.add),
                                reads=[("cmb", 0), ("ost", oslot, 0), ("ost", oslot, 1)], writes=[("cmb", 0)])
                            rden = sm[:, 8, 0:8]
                            add("dve", lambda e: e.reciprocal(out=rden, in_=cmb[0][:, :, 64]), reads=[("cmb", 0)], writes=["rden"])
                            add("dve", lambda e: e.tensor_tensor(out=onrm[:], in0=cmb[0][:, :, 0:64],
                                                                 in1=rden.unsqueeze(2).to_broadcast([128, 8, 64]), op=ALU.mult),
                                reads=[("cmb", 0), "rden"], writes=["onrm"])
                            ssa = sm[:, 8, 8:9]
                            rsa = sm[:, 8, 9:10]
                            add("act", lambda e: e.activation(out=junk[:, 0:512], in_=onrm[:].rearrange("p h c -> p (h c)"),
                                                              func=AF.Square, accum_out=ssa), reads=["onrm"], writes=["junk", "ssa"])
                            add("act", lambda e: e.activation(out=rsa, in_=ssa, func=AF.Ln, scale=1.0 / 512, bias=eps_t[:]),
                                reads=["ssa", "eps"], writes=["rsa0", "rsa"])
                            add("act", lambda e: e.activation(out=rsa, in_=rsa, func=AF.Exp, scale=-0.5), reads=["rsa0"], writes=["rsa"])
                            onb_ = (onb_p, onb_p2)[b % 2]
                            add("act", lambda e, onb_=onb_: e.activation(out=onb_[:], in_=onrm[:].rearrange("p h c -> p (h c)"), func=AF.Copy, scale=rsa),
                                reads=["onrm", "rsa"], writes=[("onb", b % 2)])
                            def back2(b=b, onb_=onb_):
                                ptm = ps[7][:].bitcast(BF16)
                                def f_tm(e):
                                    last = None
                                    for k in range(4):
                                        last = e.transpose(out=ptm[:, k * 128:(k + 1) * 128], in_=onb_[:, k * 128:(k + 1) * 128], identity=ident[:])
                                    return last
                                add("pe", f_tm, reads=[("onb", b % 2), "ident"], writes=[PS(7)])
                                add("dve", lambda e: e.tensor_tensor(
                                    out=mixA[:, :, b * 128:(b + 1) * 128], in0=ptm[:, 0:512].rearrange("p (k t) -> p k t", k=4),
                                    in1=gna[:].unsqueeze(2).to_broadcast([128, 4, 128]), op=ALU.mult),
                                    reads=[PS(7), "gna"], writes=[("mixA", b)])
                            tails.append(back2)

                    if pending is not None:
                        n_t = len(tails)
                        pending()
                        if n_t > 0:
                            tails.pop(0)()
                    pending = back
                    blk_ctr += 1
            pending()
            while tails:
                tails.pop(0)()
            S.barrier()
            if dbg and l == 0:
                dq = view(112, [128, 4, S_LEN], F32)
                add("dve", lambda e, dq=dq: e.tensor_copy(out=dq, in_=mixA), writes=["dq"])
                for cq in range(4):
                    add("sp", lambda e, cq=cq, dq=dq: e.dma_start(out=dbg_d["mixA"][:, cq, :], in_=dq[:, cq, :]), reads=["dq"], dma=True)
                S.barrier()

            if stop == 3:
                raise _Stop()
            LB = 64
            NQ = 4
            NTB = 16 * (LB + 1)
            mixS = view(80, [128, 4, S_LEN], BF16)
            bufA = view(96, [128, 16, LB], F32)
            bufB = view(100, [128, 16, LB], F32)
            bufT1 = view(104, [128, 16, LB], F32)
            bufT2 = view(108, [128, 16, LB], F32)
            tcos = view(112, [128, 16, LB + 1], F32)
            tsin = view(116.25, [128, 16, LB + 1], F32)
            Sp_re = view(120.5, [128, 16, LB + 1], BF16)
            Sp_im = view(122.75, [128, 16, LB + 1], BF16)
            Ddiag = view(125, [128, 4, 128], BF16)
            BTj = view(128, [128, 8, 2, 4, 128], BF16)
            Ct = view(144, [128, 8, 2, 16, 32], BF16)
            Dt = view(176, [128, 8, 4, 128], BF16)
            wglu = view(184, [128, 4, 512], BF16)
            prm = view(188, [128, 40, 16], F32)
            Lpw = view(190.5, [128, 2, 16, 9], F32)
            braw = view(80, [128, 2, 16, 32], F32)
            craw = view(84, [128, 2, 16, 32], F32)
            tmp = [view(88 + 2 * i, [128, 16, 32], F32) for i in range(4)]
            Bj_bf = view(96, [128, 8, 2, 16, 32], BF16)
            C32b = view(120.5, [128, 2, 16, 32], BF16)
            ang = view(144, [128, NTB], F32)
            angk = view(148.25, [128, NTB], F32)
            angi = view(152.5, [128, NTB], I32)
            a9 = view(157, [128, 144], F32)
            a9k = view(157.75, [128, 144], F32)
            a9i = view(158.5, [128, 144], I32)
            t1s = view(96, [128, 4, 512], F32)
            sqb = view(104, [128, 4, 512], BF16)

            P = lambda i: prm[:, i, :]

            def sincos(a_, ak_, ai_, o_sin, o_cos, nm):
                add("dve", lambda e: e.tensor_scalar(out=ai_, in0=a_, scalar1=1.0 / TWO_PI, scalar2=None, op0=ALU.mult),
                    reads=[nm + "a"], writes=[nm + "i"])
                add("dve", lambda e: e.tensor_copy(out=ak_, in_=ai_), reads=[nm + "i"], writes=[nm + "k"])
                add("dve", lambda e: e.tensor_scalar(out=ak_, in0=ak_, scalar1=-TWO_PI, scalar2=None, op0=ALU.mult),
                    reads=[nm + "k"], writes=[nm + "k"])
                add("dve", lambda e: e.tensor_tensor(out=a_, in0=a_, in1=ak_, op=ALU.add), reads=[nm + "a", nm + "k"], writes=[nm + "a"])
                add("dve", lambda e: e.tensor_scalar(out=ak_, in0=a_, scalar1=math.pi, scalar2=-TWO_PI, op0=ALU.is_gt, op1=ALU.mult),
                    reads=[nm + "a"], writes=[nm + "k"])
                add("dve", lambda e: e.tensor_tensor(out=a_, in0=a_, in1=ak_, op=ALU.add), reads=[nm + "a", nm + "k"], writes=[nm + "a"])
                add("dve", lambda e: e.tensor_scalar(out=ak_, in0=a_, scalar1=-math.pi, scalar2=TWO_PI, op0=ALU.is_lt, op1=ALU.mult),
                    reads=[nm + "a"], writes=[nm + "k"])
                add("dve", lambda e: e.tensor_tensor(out=a_, in0=a_, in1=ak_, op=ALU.add), reads=[nm + "a", nm + "k"], writes=[nm + "a"])
                add("act", lambda e: e.activation(out=o_sin, in_=a_, func=AF.Sin), reads=[nm + "a"], writes=[nm + "sin"])
                add("dve", lambda e: e.tensor_scalar(out=ak_, in0=a_, scalar1=math.pi / 2, scalar2=-TWO_PI, op0=ALU.is_gt, op1=ALU.mult),
                    reads=[nm + "a"], writes=[nm + "k"])
                add("dve", lambda e: e.tensor_tensor(out=ak_, in0=ak_, in1=a_, op=ALU.add), reads=[nm + "a", nm + "k"], writes=[nm + "k"])
                add("act", lambda e: e.activation(out=o_cos, in_=ak_, func=AF.Sin, bias=halfpi[:]), reads=[nm + "k", "halfpi"], writes=[nm + "cos"])

            add("sp", lambda e, l=l: e.dma_start(out=P(0), in_=lamre_d[l]), writes=["p0"], dma=True)
            add("sp", lambda e, l=l: e.dma_start(out=P(1), in_=lamim_d[l]), writes=["p1"], dma=True)
            add("sp", lambda e, l=l: e.dma_start(out=P(2), in_=logdt_d[l]), writes=["p2"], dma=True)
            add("sp", lambda e, l=l: e.dma_start(out=braw[:, 0], in_=bre_d[l]), writes=["braw0"], dma=True)
            add("sp", lambda e, l=l: e.dma_start(out=braw[:, 1], in_=bim_d[l]), writes=["braw1"], dma=True)
            add("sp", lambda e, l=l: e.dma_start(out=craw[:, 0], in_=cre_d[l]), writes=["craw0"], dma=True)
            add("sp", lambda e, l=l: e.dma_start(out=craw[:, 1], in_=cim_d[l]), writes=["craw1"], dma=True)
            load_w_cast(wglu[:], w_glu_d[l].rearrange("(k p) n -> p k n", p=128), "wglu")
            add("pool", lambda e: e.memset(Dt[:], 0.0), writes=["Dt"])
            for c in range(4):
                add("pool", lambda e, c=c: e.tensor_scalar(out=Ddiag[:, c, :], in0=identf[:], scalar1=dsk[:, c:c + 1], scalar2=None, op0=ALU.mult),
                    reads=["identf", "dsk"], writes=["Ddiag"])
            add("act", lambda e: e.activation(out=P(3), in_=P(2), func=AF.Exp), reads=["p2"], writes=["p3"])
            add("dve", lambda e: e.tensor_tensor(out=P(4), in0=P(1), in1=P(3), op=ALU.mult), reads=["p1", "p3"], writes=["p4"])
            add("dve", lambda e: e.tensor_tensor(out=P(5), in0=P(0), in1=P(3), op=ALU.mult), reads=["p0", "p3"], writes=["p5"])
            add("dve", lambda e: e.tensor_scalar(out=P(7), in0=P(4), scalar1=8.0, scalar2=None, op0=ALU.mult), reads=["p4"], writes=["p7"])
            add("act", lambda e: e.activation(out=P(8), in_=P(5), func=AF.Exp, scale=8.0), reads=["p5"], writes=["p8"])
            a9v = a9.rearrange("p (j k) -> p j k", j=16)
            a9kv = a9k.rearrange("p (j k) -> p j k", j=16)
            io9 = iota_t[:, 0:9].unsqueeze(1).to_broadcast([128, 16, 9])
            add("dve", lambda e: e.tensor_tensor(out=a9kv, in0=P(5).unsqueeze(2).to_broadcast([128, 16, 9]), in1=io9, op=ALU.mult),
                reads=["p5", "iota"], writes=["rk0"])
            rk = tmp[2][:, :, 0:9]
            add("act", lambda e: e.activation(out=rk, in_=a9kv, func=AF.Exp), reads=["rk0"], writes=["rk"])
            add("dve", lambda e: e.tensor_tensor(out=a9v, in0=P(4).unsqueeze(2).to_broadcast([128, 16, 9]), in1=io9, op=ALU.mult),
                reads=["p4", "iota", "rk"], writes=["n9a"])
            s9 = view(88, [128, 144], F32)
            c9 = view(90, [128, 144], F32)
            sincos(a9, a9k, a9i, s9, c9, "n9")
            s9v = s9.rearrange("p (j k) -> p j k", j=16)
            c9v = c9.rearrange("p (j k) -> p j k", j=16)
            add("dve", lambda e: e.tensor_tensor(out=Lpw[:, 0], in0=c9v, in1=rk, op=ALU.mult), reads=["n9cos", "rk"], writes=["Lre"])
            add("dve", lambda e: e.tensor_tensor(out=Lpw[:, 1], in0=s9v, in1=rk, op=ALU.mult), reads=["n9sin", "rk"], writes=["Lim"])
            ang3 = ang.rearrange("p (j t) -> p j t", j=16)
            add("dve", lambda e: e.tensor_tensor(out=ang3, in0=P(7).unsqueeze(2).to_broadcast([128, 16, LB + 1]),
                                                  in1=iota_t[:, 0:LB + 1].unsqueeze(1).to_broadcast([128, 16, LB + 1]), op=ALU.mult),
                reads=["p7", "iota"], writes=["nTa"])
            sincos(ang, angk, angi, tsin.rearrange("p j t -> p (j t)"), tcos.rearrange("p j t -> p (j t)"), "nT")
            S.barrier()
            if stop == 31:
                raise _Stop()
            Lre = lambda k: Lpw[:, 0, :, k]
            Lim = lambda k: Lpw[:, 1, :, k]
            bc32 = lambda ap_: ap_.unsqueeze(2).to_broadcast([128, 16, 32])
            add("dve", lambda e: e.tensor_scalar(out=P(9), in0=Lre(1), scalar1=-1.0, scalar2=None, op0=ALU.add), reads=["Lre"], writes=["p9"])
            add("dve", lambda e: e.tensor_tensor(out=P(10), in0=P(0), in1=P(0), op=ALU.mult), reads=["p0"], writes=["p10"])
            add("dve", lambda e: e.tensor_tensor(out=P(11), in0=P(1), in1=P(1), op=ALU.mult), reads=["p1"], writes=["p11"])
            add("dve", lambda e: e.tensor_tensor(out=P(10), in0=P(10), in1=P(11), op=ALU.add), reads=["p10", "p11"], writes=["p10"])
            add("dve", lambda e: e.reciprocal(out=P(10), in_=P(10)), reads=["p10"], writes=["p10"])
            add("dve", lambda e: e.tensor_tensor(out=P(11), in0=P(9), in1=P(0), op=ALU.mult), reads=["p9", "p0"], writes=["p11"])
            add("dve", lambda e: e.tensor_tensor(out=P(12), in0=Lim(1), in1=P(1), op=ALU.mult), reads=["Lim", "p1"], writes=["p12"])
            add("dve", lambda e: e.tensor_tensor(out=P(11), in0=P(11), in1=P(12), op=ALU.add), reads=["p11", "p12"], writes=["p11"])
            add("dve", lambda e: e.tensor_tensor(out=P(11), in0=P(11), in1=P(10), op=ALU.mult), reads=["p11", "p10"], writes=["p11"])
            add("dve", lambda e: e.tensor_tensor(out=P(13), in0=Lim(1), in1=P(0), op=ALU.mult), reads=["Lim", "p0"], writes=["p13"])
            add("dve", lambda e: e.tensor_tensor(out=P(14), in0=P(9), in1=P(1), op=ALU.mult), reads=["p9", "p1"], writes=["p14"])
            add("dve", lambda e: e.tensor_tensor(out=P(13), in0=P(13), in1=P(14), op=ALU.subtract), reads=["p13", "p14"], writes=["p13"])
            add("dve", lambda e: e.tensor_tensor(out=P(13), in0=P(13), in1=P(10), op=ALU.mult), reads=["p13", "p10"], writes=["p13"])
            add("dve", lambda e: e.tensor_tensor(out=tmp[2], in0=braw[:, 0], in1=bc32(P(11)), op=ALU.mult), reads=["braw0", "p11", "rk", "n9sin", "n9cos"], writes=["tmp2"])
            add("dve", lambda e: e.tensor_tensor(out=tmp[3], in0=braw[:, 1], in1=bc32(P(13)), op=ALU.mult), reads=["braw1", "p13"], writes=["tmp3"])
            add("dve", lambda e: e.tensor_tensor(out=tmp[0], in0=tmp[2], in1=tmp[3], op=ALU.subtract), reads=["tmp2", "tmp3", "Lre", "Lim"], writes=["bb_re"])
            add("dve", lambda e: e.tensor_tensor(out=tmp[2], in0=braw[:, 1], in1=bc32(P(11)), op=ALU.mult), reads=["braw1", "p11", "bb_re"], writes=["tmp2"])
            add("dve", lambda e: e.tensor_tensor(out=tmp[3], in0=braw[:, 0], in1=bc32(P(13)), op=ALU.mult), reads=["braw0", "p13", "bb_re"], writes=["tmp3"])
            add("dve", lambda e: e.tensor_tensor(out=tmp[1], in0=tmp[2], in1=tmp[3], op=ALU.add), reads=["tmp2", "tmp3"], writes=["bb_im"])
            tA, tB = tmp[2], tmp[3]
            tC, tD = braw[:, 0], braw[:, 1]
            for j in range(8):
                k = 7 - j
                add("dve", lambda e, k=k: e.tensor_tensor(out=tA, in0=tmp[0], in1=bc32(Lre(k)), op=ALU.mult), reads=["bb_re", "Lre"], writes=["tmp2"])
                add("dve", lambda e, k=k: e.tensor_tensor(out=tB, in0=tmp[1], in1=bc32(Lim(k)), op=ALU.mult), reads=["bb_im", "Lim"], writes=["tmp3"])
                add("dve", lambda e, j=j: e.tensor_tensor(out=Bj_bf[:, j, 0], in0=tA, in1=tB, op=ALU.subtract), reads=["tmp2", "tmp3"], writes=[("Bj", j, 0)])
                add("dve", lambda e, k=k: e.tensor_tensor(out=tC, in0=tmp[1], in1=bc32(Lre(k)), op=ALU.mult), reads=["bb_im", "Lre", "bb_re"], writes=["braw0"])
                add("dve", lambda e, k=k: e.tensor_tensor(out=tD, in0=tmp[0], in1=bc32(Lim(k)), op=ALU.mult), reads=["bb_re", "Lim", "bb_im"], writes=["braw1"])
                add("dve", lambda e, j=j: e.tensor_tensor(out=Bj_bf[:, j, 1], in0=tC, in1=tD, op=ALU.add), reads=["braw0", "braw1"], writes=[("Bj", j, 1)])
            add("act", lambda e: e.activation(out=C32b[:, 0], in_=craw[:, 0], func=AF.Copy), reads=["craw0"], writes=["C32b0"])
            add("act", lambda e: e.activation(out=C32b[:, 1], in_=craw[:, 1], func=AF.Copy, scale=-1.0), reads=["craw1"], writes=["C32b1"])
            tE = view(120.5 + 2.0, [128, 16, 32], F32)
            tF = view(125.0 + 1.0, [128, 16, 32], F32)
            for t in range(8):
                k = t + 1
                add("dve", lambda e, k=k: e.tensor_tensor(out=tE, in0=craw[:, 0], in1=bc32(Lre(k)), op=ALU.mult), reads=["craw0", "Lre"], writes=["tE"])
                add("dve", lambda e, k=k: e.tensor_tensor(out=tF, in0=craw[:, 1], in1=bc32(Lim(k)), op=ALU.mult), reads=["craw1", "Lim"], writes=["tF"])
                add("dve", lambda e, t=t: e.tensor_tensor(out=Ct[:, t, 0], in0=tE, in1=tF, op=ALU.subtract), reads=["tE", "tF"], writes=[("Ct", t, 0)])
                add("dve", lambda e, k=k: e.tensor_tensor(out=tE, in0=craw[:, 0], in1=bc32(Lim(k)), op=ALU.mult), reads=["craw0", "Lim", ("Ct", t, 0)], writes=["tE"])
                add("dve", lambda e, k=k: e.tensor_tensor(out=tF, in0=craw[:, 1], in1=bc32(Lre(k)), op=ALU.mult), reads=["craw1", "Lre", ("Ct", t, 0)], writes=["tF"])
                add("dve", lambda e, t=t: e.scalar_tensor_tensor(out=Ct[:, t, 1], in0=tE, scalar=-1.0, in1=tF, op0=ALU.mult, op1=ALU.subtract),
                    reads=["tE", "tF"], writes=[("Ct", t, 1)])
            for j in range(8):
                for ri in range(2):
                    bank = (2 * j + ri) % 4
                    ptb = ps[bank][:].bitcast(BF16)
                    def f_tb(e, j=j, ri=ri, ptb=ptb):
                        last = None
                        for c in range(4):
                            last = e.transpose(out=ptb[:, c * 128:(c + 1) * 128],
                                               in_=Bj_bf[:, j, ri, 4 * c:4 * c + 4, :].rearrange("p a b -> p (a b)"), identity=ident[:])
                        return last
                    add("pe", f_tb, reads=[("Bj", j, ri), "ident"], writes=[PS(bank)])
                    add("act", lambda e, j=j, ri=ri, ptb=ptb: e.activation(out=BTj[:, j, ri].rearrange("p c m -> p (c m)"), in_=ptb[:, 0:512], func=AF.Copy),
                        reads=[PS(bank)], writes=[("BTj", j, ri)])
            for hb in range(2):
                def f_d(e, hb=hb):
                    last = None
                    for tl in range(4):
                        tau = 4 * hb + tl
                        jB = 7 - tau
                        for j in range(16):
                            c, q = j // 4, j % 4
                            col = tl * 128 + c * 32
                            for ri in range(2):
                                last = e.matmul(ps[4 + hb][32 * q:32 * q + 32, col:col + 32], lhsT=Bj_bf[:, jB, ri, j, :], rhs=C32b[:, ri, j, :],
                                                start=(ri == 0), stop=(ri == 1), tile_position=(0, 32 * q), skip_group_check=True)
                    return last
                add("pe", f_d, reads=[("Bj", j, ri) for j in range(8) for ri in range(2)] + ["C32b0", "C32b1"], writes=[PS(4 + hb)])
                for q in range(4):
                    add("dve", lambda e, hb=hb, q=q: e.tensor_copy(
                        out=Dt[32 * q:32 * q + 32, 4 * hb:4 * hb + 4, :, 32 * q:32 * q + 32],
                        in_=ps[4 + hb][32 * q:32 * q + 32, :].rearrange("p (t c m) -> p t c m", t=4, c=4)),
                        reads=[PS(4 + hb), "Dt"], writes=[("Dtw", hb, q)])
            add("dve", lambda e: e.tensor_tensor(out=Dt[:, 0, :, :], in0=Dt[:, 0, :, :], in1=Ddiag[:], op=ALU.add),
                reads=[("Dtw", 0, q) for q in range(4)] + ["Ddiag", "Dt"], writes=["Dt0"])
            add("dve", lambda e: e.memset(P(20), 0.0), writes=["car_re"])
            add("dve", lambda e: e.memset(P(21), 0.0), writes=["car_im"])
            S.barrier()
            if stop == 32:
                raise _Stop()
            add("pool", lambda e: e.memset(Sp_re[:, :, 0:1], 0.0), writes=["Sp_re"])
            add("pool", lambda e: e.memset(Sp_im[:, :, 0:1], 0.0), writes=["Sp_im"])
            cosT = tcos[:, :, 0:LB]
            sinT = tsin[:, :, 0:LB]
            cL = tcos[:, :, LB]
            sL = tsin[:, :, LB]
            def emit_fw(qi):
                tok0 = qi * 512
                def f_w(e, tok0=tok0):
                    last = None
                    for c in range(4):
                        for ri in range(2):
                            for jj in range(8):
                                for q in range(4):
                                    last = e.matmul(ps[q][:, (2 * c + ri) * LB:(2 * c + ri + 1) * LB],
                                                    lhsT=BTj[32 * q:32 * q + 32, jj, ri, c, :],
                                                    rhs=uT[32 * q:32 * q + 32, c, tok0 + jj:tok0 + jj + 8 * (LB - 1) + 1:8],
                                                    start=(jj == 0), stop=(jj == 7), tile_position=(32 * q, 0), skip_group_check=True)
                    return last
                add("pe", f_w, reads=[("uT", c_, qi) for c_ in range(4)], writes=[PS(q) for q in range(4)])
            emit_fw(0)
            for qi in range(NQ):
                tok0 = qi * 512
                for q in range(4):
                    pv = ps[q][:].rearrange("p (c r t) -> p c r t", c=4, r=2)
                    add("act", lambda e, pv=pv, q=q: e.activation(out=bufA[:, q:16:4, :], in_=pv[:, :, 0, :], func=AF.Copy),
                        reads=[PS(q)], writes=[("A", q)])
                    add("act", lambda e, pv=pv, q=q: e.activation(out=bufB[:, q:16:4, :], in_=pv[:, :, 1, :], func=AF.Copy),
                        reads=[PS(q)], writes=[("B", q)])
                if qi + 1 < NQ:
                    emit_fw(qi + 1)
                Aall = [("A", q) for q in range(4)]
                Ball = [("B", q) for q in range(4)]
                add("dve", lambda e: e.tensor_tensor(out=bufT1[:], in0=bufA[:], in1=cosT, op=ALU.mult), reads=Aall, writes=["T1"])
                add("dve", lambda e: e.tensor_tensor(out=bufT2[:], in0=bufB[:], in1=sinT, op=ALU.mult), reads=Ball, writes=["T2"])
                add("dve", lambda e: e.tensor_tensor(out=bufT1[:], in0=bufT1[:], in1=bufT2[:], op=ALU.add), reads=["T1", "T2"], writes=["T1"])
                add("dve", lambda e: e.tensor_tensor(out=bufT2[:], in0=bufB[:], in1=cosT, op=ALU.mult), reads=Ball + ["T1"], writes=["T2"])
                add("dve", lambda e: e.tensor_tensor(out=bufB[:], in0=bufA[:], in1=sinT, op=ALU.mult), reads=Aall + ["T2"], writes=Ball)
                add("dve", lambda e: e.tensor_tensor(out=bufT2[:], in0=bufT2[:], in1=bufB[:], op=ALU.subtract), reads=["T2"] + Ball, writes=["T2"])
                for j in range(16):
                    add("dve", lambda e, j=j: e.tensor_tensor_scan(out=bufA[:, j, :], data0=P(8)[:, j:j + 1].to_broadcast([128, LB]),
                                                                     data1=bufT1[:, j, :], initial=P(20)[:, j:j + 1], op0=ALU.mult, op1=ALU.add),
                        reads=["T1", "p8", "car_re"], writes=[("A", j % 4)])
                for j in range(16):
                    add("dve", lambda e, j=j: e.tensor_tensor_scan(out=bufB[:, j, :], data0=P(8)[:, j:j + 1].to_broadcast([128, LB]),
                                                                     data1=bufT2[:, j, :], initial=P(21)[:, j:j + 1], op0=ALU.mult, op1=ALU.add),
                        reads=["T2", "p8", "car_im"], writes=[("B", j % 4)])
                zl_re = bufA[:, :, LB - 1]
                zl_im = bufB[:, :, LB - 1]
                add("dve", lambda e: e.tensor_tensor(out=P(22), in0=zl_re, in1=cL, op=ALU.mult), reads=Aall, writes=["p22"])
                add("dve", lambda e: e.tensor_tensor(out=P(23), in0=zl_im, in1=sL, op=ALU.mult), reads=Ball, writes=["p23"])
                add("dve", lambda e: e.tensor_tensor(out=P(20), in0=P(22), in1=P(23), op=ALU.subtract), reads=["p22", "p23"], writes=["car_re"])
                add("dve", lambda e: e.tensor_tensor(out=P(22), in0=zl_re, in1=sL, op=ALU.mult), reads=Aall + ["car_re"], writes=["p22"])
                add("dve", lambda e: e.tensor_tensor(out=P(23), in0=zl_im, in1=cL, op=ALU.mult), reads=Ball + ["car_re"], writes=["p23"])
                add("dve", lambda e: e.tensor_tensor(out=P(21), in0=P(22), in1=P(23), op=ALU.add), reads=["p22", "p23"], writes=["car_im"])
                if qi > 0:
                    add("dve", lambda e: e.tensor_copy(out=Sp_re[:, :, 0], in_=Sp_re[:, :, LB]), reads=["Sp_re"], writes=["Sp_re"])
                    add("dve", lambda e: e.tensor_copy(out=Sp_im[:, :, 0], in_=Sp_im[:, :, LB]), reads=["Sp_im"], writes=["Sp_im"])
                add("dve", lambda e: e.tensor_tensor(out=bufT1[:], in0=bufA[:], in1=cosT, op=ALU.mult), reads=Aall + ["T1"], writes=["T1"])
                add("dve", lambda e: e.tensor_tensor(out=bufT2[:], in0=bufB[:], in1=sinT, op=ALU.mult), reads=Ball + ["T2"], writes=["T2"])
                add("dve", lambda e: e.tensor_tensor(out=Sp_re[:, :, 1:LB + 1], in0=bufT1[:], in1=bufT2[:], op=ALU.subtract), reads=["T1", "T2", "Sp_re"], writes=["Sp_re"])
                add("dve", lambda e: e.tensor_tensor(out=bufT1[:], in0=bufA[:], in1=sinT, op=ALU.mult), reads=Aall + ["Sp_re"], writes=["T1"])
                add("dve", lambda e: e.tensor_tensor(out=bufT2[:], in0=bufB[:], in1=cosT, op=ALU.mult), reads=Ball + ["Sp_re"], writes=["T2"])
                add("dve", lambda e: e.tensor_tensor(out=Sp_im[:, :, 1:LB + 1], in0=bufT1[:], in1=bufT2[:], op=ALU.add), reads=["T1", "T2", "Sp_im"], writes=["Sp_im"])
                for c in range(4):
                    def f_y(e, c=c, tok0=tok0):
                        last = None
                        first = True
                        for t in range(8):
                            reg = ps[4 + c][:, t * LB:(t + 1) * LB]
                            for jj in range(t + 1):
                                last = e.matmul(reg, lhsT=Dt[:, t - jj, c, :], rhs=uT[:, c, tok0 + jj:tok0 + jj + 8 * (LB - 1) + 1:8],
                                                start=first, stop=False, skip_group_check=True)
                                first = False
                        for t in range(8):
                            for ri, Sp_ in enumerate((Sp_re, Sp_im)):
                                for q in range(4):
                                    j = 4 * c + q
                                    reg = ps[4 + c][32 * q:32 * q + 32, t * LB:(t + 1) * LB]
                                    last = e.matmul(reg, lhsT=Ct[:, t, ri, j, :], rhs=Sp_[:, j, 0:LB], start=False,
                                                    stop=(t == 7 and ri == 1 and q == 3), tile_position=(0, 32 * q), skip_group_check=True)
                        return last
                    add("pe", f_y, reads=[("uT", c, qi), "Sp_re", "Sp_im", "Ddiag"], writes=[PS(4 + c)])
                    add("act", lambda e, c=c, tok0=tok0: e.activation(
                        out=mixS[:, c, tok0:tok0 + 512].rearrange("p (b t) -> p b t", t=8),
                        in_=ps[4 + c][:].rearrange("p (t b) -> p b t", t=8), func=AF.Gelu_apprx_tanh),
                        reads=[PS(4 + c)], writes=[("yg", c, qi)])
            S.barrier()
            if stop == 33:
                raise _Stop()
            load_w_cast(view(136, [128, 8, D], BF16)[:], w_out_d[l].rearrange("(k p) n -> p k n", p=128), "wout")
            t1s2 = [view(96, [128, 4, 512], F32), view(104, [128, 4, 512], F32)]
            sqb2 = [view(112, [128, 4, 512], BF16), view(116, [128, 4, 512], BF16)]
            for tb in range(4):
                tsl = slice(tb * 512, (tb + 1) * 512)
                t1s = t1s2[tb % 2]
                sqb = sqb2[tb % 2]
                sl_ = tb % 2
                def f_g(e, tsl=tsl):
                    last = None
                    for fc in range(4):
                        for k in range(4):
                            last = e.matmul(ps[fc][:], lhsT=wglu[:, k, fc * 128:(fc + 1) * 128], rhs=mixS[:, k, tsl], start=(k == 0), stop=(k == 3))
                    return last
                add("pe", f_g, reads=["wglu"] + [("yg", c, tb) for c in range(4)], writes=[PS(fc) for fc in range(4)])
                for fc in range(4):
                    add("act", lambda e, fc=fc, t1s=t1s: e.activation(out=t1s[:, fc, :], in_=ps[fc][:], func=AF.Sigmoid),
                        reads=[PS(fc)], writes=[("t1s", sl_, fc)])
                t1all = [("t1s", sl_, fc) for fc in range(4)]
                add("dve", lambda e, tsl=tsl, t1s=t1s: e.tensor_tensor(out=t1s[:], in0=t1s[:], in1=mixS[:, :, tsl], op=ALU.mult),
                    reads=t1all + [("yg", c, tb) for c in range(4)], writes=t1all)
                add("act", lambda e, t1s=t1s, sqb=sqb: e.activation(out=sqb[:], in_=t1s[:], func=AF.Square), reads=t1all, writes=[("sqb", sl_)])
                add("dve", lambda e, tsl=tsl, t1s=t1s: e.tensor_tensor(out=mixS[:, :, tsl], in0=t1s[:], in1=gns[:].unsqueeze(2).to_broadcast([128, 4, 512]), op=ALU.mult),
                    reads=t1all + ["gns"], writes=[("yg", c, tb) for c in range(4)] + [("mixS", 4 * tb + i) for i in range(4)])
                def f_ss(e, sqb=sqb, sl_=sl_):
                    last = None
                    for i in range(4):
                        for k in range(4):
                            last = e.matmul(ps[4 + sl_][:, i:i + 1], lhsT=sqb[:, k, i * 128:(i + 1) * 128], rhs=ones_b[:], start=(k == 0), stop=(k == 3),
                                            skip_group_check=True)
                    return last
                add("pe", f_ss, reads=[("sqb", sl_), "ones_b"], writes=[PS(4 + sl_)])
                add("act", lambda e, tb=tb, sl_=sl_: e.activation(out=rstd_s[:, 4 * tb:4 * tb + 4], in_=ps[4 + sl_][:, 0:4], func=AF.Copy),
                    reads=[PS(4 + sl_)], writes=[("rstd_s0", tb)])
            add("act", lambda e: e.activation(out=rstd_s[:], in_=rstd_s[:], func=AF.Sqrt, scale=1.0 / 512, bias=eps_t[:]),
                reads=[("rstd_s0", tb) for tb in range(4)] + ["eps"], writes=["rstd_s1"])
            add("dve", lambda e: e.reciprocal(out=rstd_s[:], in_=rstd_s[:]), reads=["rstd_s1"], writes=[("rstd_s", i) for i in range(NT)])
            S.barrier()
            if dbg and l == 0:
                dq = view(152, [128, 4, S_LEN], F32)
                add("dve", lambda e, dq=dq: e.tensor_copy(out=dq, in_=mixS), writes=["dq"])
                for cq in range(4):
                    add("sp", lambda e, cq=cq, dq=dq: e.dma_start(out=dbg_d["mixS"][:, cq, :], in_=dq[:, cq, :]), reads=["dq"], dma=True)
                add("sp", lambda e: e.dma_start(out=dbg_d["rs"], in_=rstd_s[:]), dma=True)
                S.barrier()

            if stop == 4:
                raise _Stop()
            wout = view(136, [128, 8, D], BF16)
            wgu = [view(112 + 12 * i, [128, 8, 512], BF16) for i in range(2)]
            wdn = [view(120 + 12 * i, [128, 2, D], BF16) for i in range(2)]
            w_gu_v = w_gu_d[l].rearrange("e (k p) n -> e p k n", p=128)
            w_dn_v = w_dn_d[l].rearrange("e (k p) n -> e p k n", p=128)
            for e_ in range(2):
                load_w_cast(wgu[e_][:], w_gu_v[e_], ("wgu", e_))
                load_w_cast(wdn[e_][:], w_dn_v[e_], ("wdn", e_))
            for t in range(NT):
                for hf in range(2):
                    bA, bS = 2 * hf, 2 * hf + 1
                    def f_o(e, t=t, hf=hf, bA=bA, bS=bS):
                        last = None
                        for k in range(4):
                            last = e.matmul(ps[bA][:], lhsT=mixA[:, k, t * 128:(t + 1) * 128], rhs=wout[:, k, hf * 512:(hf + 1) * 512],
                                            start=(k == 0), stop=(k == 3))
                        for k in range(4):
                            last = e.matmul(ps[bS][:], lhsT=mixS[:, k, t * 128:(t + 1) * 128], rhs=wout[:, 4 + k, hf * 512:(hf + 1) * 512],
                                            start=(k == 0), stop=(k == 3))
                        return last
                    add("pe", f_o, reads=["wout", ("mixA", t), ("mixS", t)], writes=[PS(bA), PS(bS)])
                    add("dve", lambda e, t=t, hf=hf, bA=bA: e.tensor_tensor(out=x[:, t, hf * 512:(hf + 1) * 512], in0=x[:, t, hf * 512:(hf + 1) * 512],
                                                                             in1=ps[bA][:], op=ALU.add), reads=[PS(bA), ("x", t)], writes=[("x", t)])
                    add("dve", lambda e, t=t, hf=hf, bS=bS: e.scalar_tensor_tensor(
                        out=x[:, t, hf * 512:(hf + 1) * 512], in0=ps[bS][:], scalar=rstd_s[:, t:t + 1], in1=x[:, t, hf * 512:(hf + 1) * 512],
                        op0=ALU.mult, op1=ALU.add), reads=[PS(bS), ("x", t), ("rstd_s", t)], writes=[("x", t)])
                    if hf == 1:
                        emit_sq(t)
            S.barrier()
            if dbg and l == 0:
                for t in range(NT):
                    add("sp", lambda e, t=t: e.dma_start(out=dbg_d["x1"][t * 128:(t + 1) * 128, :], in_=x[:, t, :]), reads=[("x", t)], dma=True)
                S.barrier()

            if stop == 5:
                raise _Stop()
            h2T = view(64, [128, 8, S_LEN], BF16)
            rmsnorm_T(h2T, ln2, "ln2", router=True, squares_done=True)
            S.barrier()
            if dbg and l == 0:
                add("sp", lambda e: e.dma_start(out=dbg_d["gates"], in_=gates[:]), dma=True)
                S.barrier()
            if stop == 6:
                raise _Stop()
            sgb = [view(168 + 0.5 * i, [128, 256], BF16) for i in range(2)]
            ab = [view(169 + 0.5 * i, [128, 256], BF16) for i in range(2)]
            aT = [view(170 + 0.5 * i, [128, 2, 128], BF16) for i in range(2)]
            steps = [(e_, t) for e_ in range(16) for t in range(NT)]
            nst = len(steps)

            def stage_G(i):
                e_, t = steps[i]
                sl = e_ % 2
                bank = i % 2
                def f(e):
                    last = None
                    for k in range(8):
                        last = e.matmul(ps[bank][:], lhsT=h2T[:, k, t * 128:(t + 1) * 128], rhs=wgu[sl][:, k, :], start=(k == 0), stop=(k == 7))
                    return last
                add("pe", f, reads=[("wgu", sl), ("hT", t)], writes=[PS(bank)])
                add("act", lambda e: e.activation(out=sgb[bank][:], in_=ps[bank][:, 0:256], func=AF.Silu), reads=[PS(bank)], writes=[("sgb", bank)])
                add("dve", lambda e: e.scalar_tensor_tensor(out=ab[bank][:], in0=ps[bank][:, 256:512], scalar=gates[:, t, e_:e_ + 1], in1=sgb[bank][:],
                                                            op0=ALU.mult, op1=ALU.mult), reads=[PS(bank), ("sgb", bank), ("gates", t)], writes=[("ab", bank)])

            def stage_T(i):
                bank = i % 2
                ptt = ps[2 + bank][:].bitcast(BF16)
                def f(e):
                    last = None
                    for fc in range(2):
                        last = e.transpose(out=ptt[:, fc * 128:(fc + 1) * 128], in_=ab[bank][:, fc * 128:(fc + 1) * 128], identity=ident[:])
                    return last
                add("pe", f, reads=[("ab", bank), "ident"], writes=[PS(2 + bank)])
                add("act", lambda e: e.activation(out=aT[bank][:].rearrange("p a b -> p (a b)"), in_=ptt[:, 0:256], func=AF.Copy),
                    reads=[PS(2 + bank)], writes=[("aT", bank)])

            def stage_D(i):
                e_, t = steps[i]
                sl = e_ % 2
                bank = i % 2
                b0 = 4 + 2 * bank
                def f(e):
                    last = None
                    for hf in range(2):
                        for fc in range(2):
                            last = e.matmul(ps[b0 + hf][:], lhsT=aT[bank][:, fc, :], rhs=wdn[sl][:, fc, hf * 512:(hf + 1) * 512],
                                            start=(fc == 0), stop=(fc == 1))
                    return last
                add("pe", f, reads=[("aT", bank), ("wdn", sl)], writes=[PS(b0), PS(b0 + 1)])
                for hf in range(2):
                    eng = "dve"
                    add(eng, lambda e, hf=hf: e.tensor_tensor(out=x[:, t, hf * 512:(hf + 1) * 512], in0=x[:, t, hf * 512:(hf + 1) * 512],
                                                              in1=ps[b0 + hf][:], op=ALU.add), reads=[PS(b0 + hf), ("x", t)], writes=[("x", t)])
                if e_ == 15 and l + 1 < L:
                    emit_sq(t)
                if t == NT - 1 and e_ + 2 < 16:
                    load_w_cast(wgu[sl][:], w_gu_v[e_ + 2], ("wgu", sl))
                    load_w_cast(wdn[sl][:], w_dn_v[e_ + 2], ("wdn", sl))

            for i in range(nst + 2):
                if i < nst:
                    stage_G(i)
                if 1 <= i <= nst:
                    stage_T(i - 1)
                if 2 <= i <= nst + 1:
                    stage_D(i - 2)
            S.barrier()

          except _Stop:
            S.barrier()

        gfin = view(64, [128, D], F32)
        add("sp", lambda e: e.dma_start(out=gfin, in_=fin_d.partition_broadcast(128)), writes=["gfin"], dma=True)
        ob = [view(72 + 4 * i, [128, D], F32) for i in range(2)]
        for t in range(NT):
            ss = sm[:, 0, 0:1]
            rs = sm[:, 0, 1:2]
            add("act", lambda e, t=t: e.activation(out=junk[:], in_=x[:, t, :], func=AF.Square, accum_out=ss),
                reads=[("x", t)], writes=["junk", "ss"])
            add("act", lambda e: e.activation(out=rs, in_=ss, func=AF.Sqrt, scale=1.0 / D, bias=eps_t[:]), reads=["ss", "eps"], writes=["rs0", "rs"])
            add("dve", lambda e: e.reciprocal(out=rs, in_=rs), reads=["rs0"], writes=["rs"])
            add("dve", lambda e, t=t: e.scalar_tensor_tensor(out=ob[t % 2], in0=x[:, t, :], scalar=rs, in1=gfin, op0=ALU.mult, op1=ALU.mult),
                reads=[("x", t), "rs", "gfin"], writes=[("ob", t % 2)])
            add("sp", lambda e, t=t: e.dma_start(out=out_d[t * 128:(t + 1) * 128, :], in_=ob[t % 2]), reads=[("ob", t % 2)], dma=True)

        S.emit(nc, es)
    return nc


def prep_inputs(inp, depth=DEPTH):
    f = lambda a: np.ascontiguousarray(np.asarray(a, dtype=np.float32))
    L = depth
    G, Pn, H = 32, 64, 16
    sh = {}
    sh["w_in"] = f(inp["w_in"][:L])
    sh["w_out"] = f(inp["w_out"][:L])
    sh["w_glu"] = f(inp["w_glu"][:L])
    sh["w_gu"] = f(np.concatenate([np.asarray(inp["w_gate"][:L]), np.asarray(inp["w_up"][:L])], axis=-1))
    sh["w_dn"] = f(inp["w_down"][:L])
    wre = np.transpose(np.asarray(inp["w_router_exp"][:L]), (0, 2, 1, 3)).reshape(L, D, 16)
    sh["w_r"] = f(np.concatenate([np.asarray(inp["w_router_grp"][:L]), wre], axis=-1))
    sh["b_r"] = f(np.concatenate([np.asarray(inp["b_router_grp"][:L]), np.asarray(inp["b_router_exp"][:L]).reshape(L, 16)], axis=-1)[:, None, :])
    pk = lambda a, k: f(np.transpose(np.asarray(a[:L]).reshape(L, k, 128), (0, 2, 1)))
    sh["ln1"] = pk(inp["ln1_g"], 8)
    sh["ln2"] = pk(inp["ln2_g"], 8)
    sh["gna"] = pk(inp["gn_attn"], 4)
    sh["gns"] = pk(inp["gn_ssm"], 4)
    sh["fin"] = f(np.asarray(inp["final_g"]).reshape(1, D))
    sh["dsk"] = pk(np.asarray(inp["d_skip"]).reshape(-1, 512), 4)
    sh["lamre"] = pk(np.asarray(inp["lam_re"]).reshape(-1, G * Pn), 16)
    sh["lamim"] = pk(np.asarray(inp["lam_im"]).reshape(-1, G * Pn), 16)
    ldt = np.repeat(np.asarray(inp["log_dt"])[:, :, None], Pn, axis=2).reshape(-1, G * Pn)
    sh["logdt"] = pk(ldt, 16)
    bre = np.zeros((L, 128, 16, 32), np.float32)
    bim = np.zeros((L, 128, 16, 32), np.float32)
    cre = np.zeros((L, 128, 16, 32), np.float32)
    cim = np.zeros((L, 128, 16, 32), np.float32)
    b_re = np.asarray(inp["b_re"]); b_im = np.asarray(inp["b_im"]); c_re = np.asarray(inp["c_re"]); c_im = np.asarray(inp["c_im"])
    for j in range(16):
        for gl in range(2):
            g = 2 * j + gl
            bre[:, gl * 64:(gl + 1) * 64, j, gl * 16:(gl + 1) * 16] = b_re[:L, g]
            bim[:, gl * 64:(gl + 1) * 64, j, gl * 16:(gl + 1) * 16] = b_im[:L, g]
            col = 16 * gl
            cre[:, gl * 64:(gl + 1) * 64, j, col:col + 16] = np.transpose(c_re[:L, g], (0, 2, 1))
            cim[:, gl * 64:(gl + 1) * 64, j, col:col + 16] = np.transpose(c_im[:L, g], (0, 2, 1))
    sh["bre"], sh["bim"], sh["cre"], sh["cim"] = bre, bim, cre, cim
    return sh


_NC_CACHE = {}


def kernel(**inputs):
    x = np.asarray(inputs["x"], dtype=np.float32)
    B = x.shape[0]
    sh = prep_inputs(inputs)
    if "nc" not in _NC_CACHE:
        _NC_CACHE["nc"] = build()
    nc = _NC_CACHE["nc"]
    in_maps = []
    for b in range(B):
        m = dict(sh)
        m["x"] = np.ascontiguousarray(x[b])
        in_maps.append(m)
    res = run_bass_kernel_spmd(nc, in_maps, core_ids=list(range(B)))
    out = np.stack([np.asarray(r["out"]) for r in res.results], axis=0)
    return out.astype(np.float32)
```
